# Optimizing a Trainium2 kernel written in Bass

```python
import jax, jax.numpy as jnp
from jax import lax
import numpy as np

D_MODEL = 1024
BATCH = 16
SEQ = 2048
DEPTH = 1

MEM_LEN = 256
HG_HEADS = 4
HG_KEY_DIM = 128
HG_VAL_DIM = 128
HG_CHUNK = 64
HG_QK = HG_HEADS * HG_KEY_DIM
HG_V = HG_HEADS * HG_VAL_DIM
SB_HEADS = 8
SB_HEAD_DIM = 64
SB_W = SB_HEADS * SB_HEAD_DIM
SB_BLOCK = 128
XA_HEADS = 4
XA_HEAD_DIM = D_MODEL // XA_HEADS
XA_W = XA_HEADS * XA_HEAD_DIM
N_GROUPS = 4
EXPERTS_PER_GROUP = 4
N_EXPERTS = N_GROUPS * EXPERTS_PER_GROUP
TOP_K_INNER = 2
EXPERT_FF = 512
IN_SPLITS = (HG_QK, HG_QK, HG_V, HG_V, SB_W, SB_W, SB_W, D_MODEL, D_MODEL)
N_IN = HG_QK * 2 + HG_V * 2 + SB_W * 3 + D_MODEL * 2
LN_EPS = 1e-5
RMS_EPS = 1e-6
DN_ALPHA = (2 * DEPTH) ** 0.25
DN_BETA = (8 * DEPTH) ** -0.25

kernel_name = "hybrid_hgrn2_stickbreaking_hmoe_deepnorm"


def layer_norm(x, g, b):
    xf = x.astype(jnp.float32)
    mu = jnp.mean(xf, axis=-1, keepdims=True)
    xc = xf - mu
    var = jnp.mean(xc * xc, axis=-1, keepdims=True)
    y = xc * lax.rsqrt(var + LN_EPS) * g.astype(jnp.float32) + b.astype(jnp.float32)
    return y.astype(x.dtype)


def hgrn2_branch(q, f_logit, i, g, lb, norm_g):
    B, S, _ = q.shape
    nc = S // HG_CHUNK
    f32 = jnp.float32

    def heads(t, d):
        t = t.astype(f32).reshape(B, nc, HG_CHUNK, HG_HEADS, d)
        return t.transpose(1, 0, 3, 2, 4)

    lbf = lb.astype(f32)
    forget = lbf + (1.0 - lbf) * jax.nn.sigmoid(f_logit.astype(f32))
    log_f = heads(jnp.log(forget), HG_KEY_DIM)
    kh = heads(1.0 - forget, HG_KEY_DIM)
    qh = heads(q, HG_KEY_DIM)
    vh = heads(i, HG_VAL_DIM)
    causal = jnp.tril(jnp.ones((HG_CHUNK, HG_CHUNK), dtype=bool))

    def step(state, xs):
        qc, kc, vc, lfc = xs
        b = jnp.cumsum(lfc, axis=-2)
        rel = b[:, :, :, None, :] - b[:, :, None, :, :]
        decay = jnp.exp(jnp.where(causal[:, :, None], rel, -jnp.inf))
        scores = jnp.einsum('bhtd,bhsd,bhtsd->bhts', qc, kc, decay)
        out = (jnp.einsum('bhts,bhsv->bhtv', scores, vc)
               + jnp.einsum('bhtd,bhdv->bhtv', qc * jnp.exp(b), state))
        b_last = b[:, :, -1:, :]
        new_state = (jnp.exp(b_last[:, :, 0, :, None]) * state
                     + jnp.einsum('bhsd,bhsv->bhdv', kc * jnp.exp(b_last - b), vc))
        return new_state, out

    s0 = jnp.zeros((B, HG_HEADS, HG_KEY_DIM, HG_VAL_DIM), f32)
    _, o = lax.scan(step, s0, (qh, kh, vh, log_f))
    o = o.transpose(1, 0, 3, 2, 4).reshape(B, S, HG_HEADS, HG_VAL_DIM)
    o = o * lax.rsqrt(jnp.mean(o * o, axis=-1, keepdims=True) + RMS_EPS) * norm_g.astype(f32)
    gate = jax.nn.silu(g.astype(f32)).reshape(B, S, HG_HEADS, HG_VAL_DIM)
    return (o * gate).reshape(B, S, HG_V).astype(q.dtype)


def stick_breaking_branch(q, k, v):
    B, S, _ = q.shape

    def heads(t):
        return t.reshape(B, S, SB_HEADS, SB_HEAD_DIM).transpose(0, 2, 1, 3)

    qh, kh, vh = heads(q), heads(k), heads(v)
    scale = SB_HEAD_DIM ** -0.5
    outs = []
    for blk in range(S // SB_BLOCK):
        t0 = blk * SB_BLOCK
        t1 = t0 + SB_BLOCK
        z = jnp.einsum('bhtd,bhsd->bhts', qh[:, :, t0:t1], kh[:, :, :t1]).astype(jnp.float32) * scale
        before = jnp.arange(t1)[None, :] < (t0 + jnp.arange(SB_BLOCK))[:, None]
        log_keep = jnp.where(before, jax.nn.log_sigmoid(-z), 0.0)
        shifted = jnp.pad(log_keep[..., 1:], ((0, 0), (0, 0), (0, 0), (0, 1)))
        tail = lax.cumsum(shifted, axis=3, reverse=True)
        weights = jnp.where(before, jnp.exp(jax.nn.log_sigmoid(z) + tail), 0.0)
        outs.append(jnp.einsum('bhts,bhsd->bhtd', weights.astype(vh.dtype), vh[:, :, :t1]))
    o = jnp.concatenate(outs, axis=2)
    return o.transpose(0, 2, 1, 3).reshape(B, S, SB_W)


def memory_cross_attention(h, mem, wq, wk, wv, wo):
    B, S, _ = h.shape
    M = mem.shape[1]
    q = (h @ wq).reshape(B, S, XA_HEADS, XA_HEAD_DIM)
    k = (mem @ wk).reshape(B, M, XA_HEADS, XA_HEAD_DIM)
    v = (mem @ wv).reshape(B, M, XA_HEADS, XA_HEAD_DIM)
    s = jnp.einsum('bshd,bmhd->bhsm', q, k).astype(jnp.float32) * (XA_HEAD_DIM ** -0.5)
    p = jax.nn.softmax(s, axis=-1).astype(v.dtype)
    o = jnp.einsum('bhsm,bmhd->bshd', p, v).reshape(B, S, XA_W)
    return o @ wo


def hierarchical_moe(h, wg, bg, we, be, w1, w3, w2):
    B, S, D = h.shape
    T = B * S
    hf = h.reshape(T, D)
    g_prob = jax.nn.softmax((hf @ wg).astype(jnp.float32) + bg.astype(jnp.float32), axis=-1)
    g_top, g_idx = lax.top_k(g_prob, 1)
    e_logits = ((hf @ we).astype(jnp.float32) + be.astype(jnp.float32)).reshape(T, N_GROUPS, EXPERTS_PER_GROUP)
    sel_idx = jnp.broadcast_to(g_idx[:, :, None], (T, 1, EXPERTS_PER_GROUP))
    in_group = jnp.take_along_axis(e_logits, sel_idx, axis=1)[:, 0]
    e_prob = jax.nn.softmax(in_group, axis=-1)
    e_top, e_idx = lax.top_k(e_prob, TOP_K_INNER)
    gate_w = g_top * (e_top / jnp.sum(e_top, axis=-1, keepdims=True))
    expert_ids = g_idx * EXPERTS_PER_GROUP + e_idx
    combine = jnp.einsum('tk,tke->te', gate_w,
                         jax.nn.one_hot(expert_ids, N_EXPERTS, dtype=jnp.float32)).astype(hf.dtype)
    y = jnp.zeros_like(hf)
    for e in range(N_EXPERTS):
        hidden = jax.nn.silu(hf @ w1[e]) * (hf @ w3[e])
        y = y + combine[:, e:e + 1] * (hidden @ w2[e])
    return y.reshape(B, S, D)


def setup_inputs(seed: int = 0) -> dict:
    key = jax.random.key(seed)
    ks = jax.random.split(key, 32)
    f32 = jnp.float32

    def nrm(k, shape, scale):
        return jax.random.normal(k, shape, f32) * scale

    def gain(k, shape):
        return jnp.ones(shape, f32) + 0.02 * jax.random.normal(k, shape, f32)

    return {
        "x": nrm(ks[0], (BATCH, SEQ, D_MODEL), 1.0),
        "mem": nrm(ks[1], (BATCH, MEM_LEN, D_MODEL), 1.0),
        "ln_in_g": gain(ks[2], (D_MODEL,)),
        "ln_in_b": nrm(ks[3], (D_MODEL,), 0.02),
        "w_in": nrm(ks[4], (DEPTH, D_MODEL, N_IN), D_MODEL ** -0.5),
        "hg_lb_logits": nrm(ks[5], (DEPTH + 1, HG_QK), 0.1),
        "hg_norm_g": gain(ks[6], (DEPTH, HG_VAL_DIM)),
        "w_branch_a": nrm(ks[7], (DEPTH, HG_V, D_MODEL), HG_V ** -0.5 * DN_BETA),
        "w_branch_b": nrm(ks[8], (DEPTH, SB_W, D_MODEL), SB_W ** -0.5 * DN_BETA),
        "w_mix_out": nrm(ks[9], (DEPTH, D_MODEL, D_MODEL), D_MODEL ** -0.5 * DN_BETA),
        "ln1_g": gain(ks[10], (DEPTH, D_MODEL)),
        "ln1_b": nrm(ks[11], (DEPTH, D_MODEL), 0.02),
        "xa_wq": nrm(ks[12], (DEPTH, D_MODEL, XA_W), D_MODEL ** -0.5),
        "xa_wk": nrm(ks[13], (DEPTH, D_MODEL, XA_W), D_MODEL ** -0.5),
        "xa_wv": nrm(ks[14], (DEPTH, D_MODEL, XA_W), D_MODEL ** -0.5 * DN_BETA),
        "xa_wo": nrm(ks[15], (DEPTH, XA_W, D_MODEL), XA_W ** -0.5 * DN_BETA),
        "ln2_g": gain(ks[16], (DEPTH, D_MODEL)),
        "ln2_b": nrm(ks[17], (DEPTH, D_MODEL), 0.02),
        "router_wg": nrm(ks[18], (DEPTH, D_MODEL, N_GROUPS), D_MODEL ** -0.5),
        "router_bg": nrm(ks[19], (DEPTH, N_GROUPS), 0.01),
        "router_we": nrm(ks[20], (DEPTH, D_MODEL, N_EXPERTS), D_MODEL ** -0.5),
        "router_be": nrm(ks[21], (DEPTH, N_EXPERTS), 0.01),
        "moe_w1": nrm(ks[22], (DEPTH, N_EXPERTS, D_MODEL, EXPERT_FF), D_MODEL ** -0.5),
        "moe_w3": nrm(ks[23], (DEPTH, N_EXPERTS, D_MODEL, EXPERT_FF), D_MODEL ** -0.5),
        "moe_w2": nrm(ks[24], (DEPTH, N_EXPERTS, EXPERT_FF, D_MODEL), EXPERT_FF ** -0.5 * DN_BETA),
        "ln3_g": gain(ks[25], (DEPTH, D_MODEL)),
        "ln3_b": nrm(ks[26], (DEPTH, D_MODEL), 0.02),
    }


def reference(x, mem, ln_in_g, ln_in_b, w_in, hg_lb_logits, hg_norm_g, w_branch_a, w_branch_b,
              w_mix_out, ln1_g, ln1_b, xa_wq, xa_wk, xa_wv, xa_wo, ln2_g, ln2_b,
              router_wg, router_bg, router_we, router_be, moe_w1, moe_w3, moe_w2, ln3_g, ln3_b):
    lower_bounds = jnp.cumsum(jax.nn.softmax(hg_lb_logits.astype(jnp.float32), axis=0), axis=0)
    split_at = list(np.cumsum(IN_SPLITS)[:-1])
    h = layer_norm(x, ln_in_g, ln_in_b)
    for l in range(DEPTH):
        proj = h @ w_in[l]
        q_hg, f_hg, i_hg, g_hg, q_sb, k_sb, v_sb, gate_a, gate_b = jnp.split(proj, split_at, axis=-1)
        y_a = hgrn2_branch(q_hg, f_hg, i_hg, g_hg, lower_bounds[l], hg_norm_g[l]) @ w_branch_a[l]
        y_b = stick_breaking_branch(q_sb, k_sb, v_sb) @ w_branch_b[l]
        merged = jax.nn.sigmoid(gate_a) * y_a + jax.nn.sigmoid(gate_b) * y_b
        h = layer_norm(DN_ALPHA * h + merged @ w_mix_out[l], ln1_g[l], ln1_b[l])
        h = layer_norm(DN_ALPHA * h + memory_cross_attention(h, mem, xa_wq[l], xa_wk[l], xa_wv[l], xa_wo[l]),
                       ln2_g[l], ln2_b[l])
        moe_out = hierarchical_moe(h, router_wg[l], router_bg[l], router_we[l], router_be[l],
                                   moe_w1[l], moe_w3[l], moe_w2[l])
        h = layer_norm(DN_ALPHA * h + moe_out, ln3_g[l], ln3_b[l])
    return h
```

```python
import os
import numpy as np
from contextlib import ExitStack
import concourse.bass as bass
import concourse.mybir as mybir
from concourse.bass_utils import run_bass_kernel_spmd

F32 = mybir.dt.float32
BF16 = mybir.dt.bfloat16
AF = mybir.ActivationFunctionType
ALU = mybir.AluOpType
AX = mybir.AxisListType

D = 1024
ALPHA = 2.0 ** 0.25
NCST = 2304


class Buf:
    __slots__ = ("w", "r")

    def __init__(self):
        self.w = None
        self.r = {}


class K:
    def __init__(self, nc, es):
        self.nc = nc
        self.es = es
        self.E = {"pe": nc.tensor, "act": nc.scalar, "dve": nc.vector, "pool": nc.gpsimd, "sp": nc.sync}
        self.sems = {}
        self.cnt = {}
        self.waited = {}
        for e in self.E:
            self.sems[e] = es.enter_context(nc.semaphore("s_" + e))
            self.cnt[e] = 0
        self.nd = 0
        self.INLINE = set(os.environ.get("KINLINE", "act,dve,pool,pe").split(","))
        self._pe_first = None
        pe = nc.tensor
        o_mm, o_tr = pe.matmul, pe.transpose

        def mm(*a, **kw):
            ins = o_mm(*a, **kw)
            if self._pe_first is None:
                self._pe_first = ins
            return ins

        def tr(*a, **kw):
            ins = o_tr(*a, **kw)
            if self._pe_first is None:
                self._pe_first = ins
            return ins
        pe.matmul = mm
        pe.transpose = tr

    def dsem(self, name):
        key = "d_%s_%d" % (name, self.nd)
        self.nd += 1
        self.sems[key] = self.es.enter_context(self.nc.semaphore(key))
        self.cnt[key] = 0
        return key

    def sb(self, name, shape, dt):
        return self.es.enter_context(self.nc.sbuf_tensor("sb_" + name, list(shape), dt))

    def ps(self, name, shape, dt=F32):
        return self.es.enter_context(self.nc.psum_tensor(name, list(shape), dt))

    def op(self, eng, fn, reads=(), writes=(), dma=None):
        def flat(xs):
            out = []
            for x in xs:
                if isinstance(x, (tuple, list)):
                    out.extend(flat(x))
                else:
                    out.append(x)
            return out
        reads = flat(reads)
        writes = flat(writes)
        deps = {}
        for b in reads:
            if b.w is not None:
                kk, v = b.w
                if deps.get(kk, 0) < v:
                    deps[kk] = v
        for b in writes:
            if b.w is not None:
                kk, v = b.w
                if deps.get(kk, 0) < v:
                    deps[kk] = v
            for kk, v in b.r.items():
                if deps.get(kk, 0) < v:
                    deps[kk] = v
        E = self.E[eng]
        need = [(kk, v) for kk, v in deps.items() if self.waited.get((eng, kk), 0) < v]
        inline = None
        if need and dma is None and eng in self.INLINE:
            inline = need.pop()
        for kk, v in need:
            E.wait_ge(self.sems[kk], v)
            self.waited[(eng, kk)] = v
        self._pe_first = None
        ins = fn()
        if inline is not None:
            tgt = self._pe_first if eng == "pe" else ins
            tgt._wait_ge(self.sems[inline[0]], inline[1])
            self.waited[(eng, inline[0])] = inline[1]
        if dma is None:
            key = eng
            self.cnt[key] += 1
            ins.then_inc(self.sems[key], 1)
        else:
            key = dma
            self.cnt[key] += 16
            ins.then_inc(self.sems[key], 16)
        v = self.cnt[key]
        for b in reads:
            if b.r.get(key, 0) < v:
                b.r[key] = v
        for b in writes:
            b.w = (key, v)
            b.r = {}
        return (key, v)

    def barrier(self):
        for eng, E in self.E.items():
            for kk, v in self.cnt.items():
                if v > 0 and self.waited.get((eng, kk), 0) < v:
                    E.wait_ge(self.sems[kk], v)
                    self.waited[(eng, kk)] = v


def make_consts():
    c = np.zeros((128, NCST), np.float32)
    j = np.arange(128)[:, None]
    s = np.arange(128)[None, :]
    c[:, 0:128] = np.eye(128)
    c[:, 128:256] = (j >= s)
    c[:, 256:384] = (j < s)
    c[:, 384:512] = (j < s)
    c[:, 512:640] = (j <= s) & ((j // 64) == (s // 64))
    t = np.arange(512)[None, :]
    c[:, 640:1152] = (t % 64 != 0)
    c[:, 1152:1664] = ((t // 64) % 2 == 0)
    c[:, 1664:2176] = ((t // 64) % 2 == 1)
    c[:, 2176:2304] = 1.0
    return c


def build(S=2048, NB=2, stop=None):
    NT = S // 128
    NG = S // 512
    nc = bass.Bass("TRN2", target_bir_lowering=False)

    def din(name, shape):
        return nc.dram_tensor(name, list(shape), F32, kind="ExternalInput").ap()

    x = din("x", [NB, S, D])
    mem = din("mem", [NB, 256, D])
    w_in = din("w_in", [D, 5632])
    wa_d = din("w_branch_a", [512, D])
    wb_d = din("w_branch_b", [512, D])
    wmix_d = din("w_mix_out", [D, D])
    wq_d = din("xa_wq", [D, D])
    wk_d = din("xa_wk", [D, D])
    wv_d = din("xa_wv", [D, D])
    wo_d = din("xa_wo", [D, D])
    w1_d = din("moe_w1", [16, D, 512])
    w3_d = din("moe_w3", [16, D, 512])
    w2_d = din("moe_w2", [16, 512, D])
    cst_d = din("cst", [128, NCST])
    colv_d = din("colv", [128, 56])
    rowv_d = din("rowv", [8, D])
    ng_d = din("ng", [1, 128])
    wr_d = din("wr", [D, 20])
    br_d = din("br", [1, 20])
    y = nc.dram_tensor("y", [NB, S, D], F32, kind="ExternalOutput").ap()
    dbg = None
    if stop is not None:
        dbg = nc.dram_tensor("dbg", [128, NT * 1024], F32, kind="ExternalOutput").ap()
        dbg2 = nc.dram_tensor("dbg2", [128, 8 * S], BF16, kind="ExternalOutput").ap()

    with ExitStack() as es:
        k = K(nc, es)
        T, V, A, G_, SPQ = nc.tensor, nc.vector, nc.scalar, nc.gpsimd, nc.sync

        SCR = 44 * 1024
        scr = k.sb("scr", [128, SCR // 4], F32)
        scrb = scr[:].bitcast(BF16)
        cst = scr[:, 0:NCST]
        Bcst0 = Buf()
        Bcst = Buf()
        cpf = k.sb("cpf", [128, 898], F32)
        idf = cpf[:, 0:128]
        dmask = cpf[:, 128:256]
        hmask = cpf[:, 256:384]
        resetm = cpf[:, 384:896]
        mlo = cpf[:, 896:897]
        mhi = cpf[:, 897:898]
        cbf = k.sb("cbf", [128, 1792], BF16)
        ident_bf = cbf[:, 1536:1664]
        neg_bf = cbf[:, 1664:1792]
        L_bf = cbf[:, 0:128]
        U_bf = cbf[:, 128:256]
        ones_bf = cbf[:, 256:384]
        zeros_bf = cbf[:, 384:512]
        evenm = cbf[:, 512:1024]
        oddm = cbf[:, 1024:1536]
        colv = k.sb("colv", [128, 56], F32)
        lbt = k.sb("lbt", [128, 16], F32)
        oml = lbt[:, 12:16]
        agbc = k.sb("agbc", [128, D], F32)
        Bagbc, Bg3 = Buf(), Buf()
        abbc = k.sb("abbc", [128, D], F32)
        cmbA = k.sb("cmbA", [128, NT, 16], F32)
        BcmbA = [Buf() for _ in range(NT)]
        ngbc4 = k.sb("ngbc4", [128, 512], F32)
        wr = scr[:, NCST:NCST + 160].rearrange("p (c n) -> p c n", c=8)
        brbc = k.sb("brbc", [128, 20], F32)
        wrb = k.sb("wrb", [128, 8, 20], BF16)
        hT = k.sb("hT", [128, 8, S], BF16)
        BhT = [(Buf(), Buf()) for _ in range(NG)]
        arena = k.sb("arena", [128, NT * 1024], F32)
        abf = arena[:].bitcast(BF16)
        oaT = abf[:, 0:4 * S].rearrange("p (c s) -> p c s", c=4)
        obT = abf[:, 4 * S:8 * S].rearrange("p (c s) -> p c s", c=4)
        mrg = abf[:, 8 * S:16 * S].rearrange("p (i c t) -> p i c t", i=NT, c=8)
        Bxn = [Buf() for _ in range(NT)]
        Boa = [Buf() for _ in range(NG)]
        Bob = [Buf() for _ in range(NG)]
        Bmrg = [Buf() for _ in range(NT)]
        NSLOT = 6
        wsl = [k.sb("wsl%d" % i, [128, 4096], BF16) for i in range(NSLOT)]
        Bw = [Buf() for _ in range(NSLOT)]
        dw = [k.dsem("w") for _ in range(NSLOT)]
        NST = 4
        stt = [k.sb("st%d" % i, [128, 32], F32) for i in range(NST)]
        Bst = [Buf() for _ in range(NST)]
        stc = [0]
        P = [k.ps("ps%d" % i, [128, 512], F32) for i in range(8)]
        BP = [Buf() for _ in range(8)]
        pc = [0]

        def nextP():
            i = pc[0] % 8
            pc[0] += 1
            return P[i], BP[i]

        dld = k.dsem("ld")
        dout = k.dsem("out")
        dxr = [k.dsem("xr") for _ in range(4)]
        douts = [k.dsem("o0"), k.dsem("o1")]

        def wload(slot, src, kc):
            view = wsl[slot][:].rearrange("p (c n) -> p c n", c=kc)
            k.op("pool", lambda: G_.dma_start(out=view, in_=src.rearrange("(c p) n -> p c n", p=128)),
                 writes=[Bw[slot]], dma=dw[slot])
            return view

        k.op("sp", lambda: SPQ.dma_start(out=cst, in_=cst_d), writes=[Bcst0], dma=k.dsem("cst"))
        k.op("act", lambda: A.copy(out=cpf[:, 0:128], in_=cst[:, 0:128]), reads=[Bcst0], writes=[Bcst])
        k.op("dve", lambda: V.tensor_copy(out=cpf[:, 128:384], in_=cst[:, 384:640]), reads=[Bcst0], writes=[Bcst])
        k.op("act", lambda: A.copy(out=cpf[:, 384:896], in_=cst[:, 640:1152]), reads=[Bcst0], writes=[Bcst])
        k.op("dve", lambda: V.tensor_copy(out=cpf[:, 896:897], in_=cst[:, 320:321]), reads=[Bcst0], writes=[Bcst])
        k.op("dve", lambda: V.tensor_copy(out=cpf[:, 897:898], in_=cst[:, 192:193]), reads=[Bcst0], writes=[Bcst])
        Bcol = Buf()
        k.op("sp", lambda: SPQ.dma_start(out=colv[:], in_=colv_d), writes=[Bcol], dma=k.dsem("colv"))
        Bwr = Buf()
        Bbrr = Buf()
        k.op("sp", lambda: SPQ.dma_start(out=wr, in_=wr_d.rearrange("(c p) n -> p c n", p=128)), writes=[Bwr], dma=k.dsem("wr"))
        k.op("sp", lambda: SPQ.dma_start(out=brbc[:], in_=br_d.partition_broadcast(128)), writes=[Bbrr], dma=k.dsem("brr"))
        k.op("act", lambda: A.copy(out=wrb[:], in_=wr), reads=[Bwr], writes=[Bwr])
        Bng = Buf()
        dng = k.dsem("ng")
        for hh in range(4):
            k.op("sp", lambda hh=hh: SPQ.dma_start(out=ngbc4[:, hh * 128:(hh + 1) * 128], in_=ng_d.partition_broadcast(128)),
                 writes=[Bng], dma=dng)
        dag, dab, dg3, db3 = k.dsem("ag"), k.dsem("ab"), k.dsem("g3"), k.dsem("b3")
        Babbc, Bb3 = Buf(), Buf()
        Bcbf = Buf()
        k.op("act", lambda: A.copy(out=cbf[:, 0:256], in_=cst[:, 128:384]), reads=[Bcst0], writes=[Bcbf])
        k.op("act", lambda: A.copy(out=cbf[:, 256:384], in_=cst[:, 2176:2304]), reads=[Bcst0], writes=[Bcbf])
        k.op("dve", lambda: V.memset(cbf[:, 384:512], 0.0), writes=[Bcbf])
        k.op("act", lambda: A.copy(out=cbf[:, 512:1536], in_=cst[:, 1152:2176]), reads=[Bcst0], writes=[Bcbf])
        k.op("act", lambda: A.copy(out=cbf[:, 1536:1664], in_=cst[:, 0:128]), reads=[Bcst0], writes=[Bcbf])
        k.op("dve", lambda: V.tensor_scalar(out=cbf[:, 1664:1792], in0=cst[:, 384:512], scalar1=-1.0, scalar2=30000.0, op0=ALU.add, op1=ALU.mult),
             reads=[Bcst0], writes=[Bcbf])
        k.op("act", lambda: A.activation(out=lbt[:, 0:8], in_=colv[:, 48:56], func=AF.Exp), reads=[Bcol], writes=[Bcol])
        k.op("dve", lambda: V.tensor_tensor(out=lbt[:, 8:12], in0=lbt[:, 0:4], in1=lbt[:, 4:8], op=ALU.add), reads=[Bcol], writes=[Bcol])
        k.op("dve", lambda: V.reciprocal(out=lbt[:, 8:12], in_=lbt[:, 8:12]), reads=[Bcol], writes=[Bcol])
        k.op("dve", lambda: V.tensor_tensor(out=lbt[:, 12:16], in0=lbt[:, 4:8], in1=lbt[:, 8:12], op=ALU.mult), reads=[Bcol], writes=[Bcol])

        def load_resid_consts(l):
            k.op("sp", lambda: SPQ.dma_start(out=agbc[:], in_=rowv_d[l:l + 1, :].partition_broadcast(128)), writes=[Bagbc], dma=dag)
            k.op("act", lambda: A.mul(out=agbc[:], in_=agbc[:], mul=ALPHA), reads=[Bagbc], writes=[Bagbc])
            k.op("sp", lambda: SPQ.dma_start(out=abbc[:], in_=rowv_d[5 + l:6 + l, :].partition_broadcast(128)), writes=[Babbc], dma=dab)
            k.op("act", lambda: A.mul(out=abbc[:], in_=abbc[:], mul=ALPHA), reads=[Babbc], writes=[Babbc])

        def ln_stats(src, Bsrc):
            i = stc[0] % NST
            stc[0] += 1
            st, B = stt[i], Bst[i]
            k.op("dve", lambda: V.bn_stats(out=st[:, 0:6], in_=src[:, 0:512]), reads=[Bsrc], writes=[B])
            k.op("dve", lambda: V.bn_stats(out=st[:, 6:12], in_=src[:, 512:1024]), reads=[Bsrc], writes=[B])
            k.op("dve", lambda: V.bn_aggr(out=st[:, 12:14], in_=st[:, 0:12]), reads=[B], writes=[B])
            k.op("act", lambda: A.activation(out=st[:, 14:15], in_=st[:, 13:14], func=AF.Ln, bias=epsc[:, 0:1], scale=1.0), reads=[B, Beps], writes=[B])
            k.op("act", lambda: A.activation(out=st[:, 15:16], in_=st[:, 14:15], func=AF.Exp, scale=-0.5), reads=[B], writes=[B])
            k.op("dve", lambda: V.scalar_tensor_tensor(out=st[:, 16:17], in0=st[:, 12:13], scalar=-1.0, in1=st[:, 15:16],
                                                      op0=ALU.mult, op1=ALU.mult), reads=[B], writes=[B])
            return st[:, 15:16], st[:, 16:17], B

        epsc = k.sb("epsc", [128, 4], F32)
        Beps = Buf()
        k.op("dve", lambda: V.memset(epsc[:, 0:1], 1e-5), writes=[Beps])
        k.op("dve", lambda: V.memset(epsc[:, 1:2], 1e-6), writes=[Beps])
        k.op("dve", lambda: V.memset(epsc[:, 2:3], 1.0), writes=[Beps])

        def transpose_evac(src, Bsrc, l, i, router=None):
            g = i // 4
            tok = slice(i * 128, (i + 1) * 128)
            for half in range(2):
                pb, Bpb = nextP()

                def tr():
                    for j in range(4):
                        c = half * 4 + j
                        ins = T.transpose(out=pb[:, j * 128:(j + 1) * 128], in_=src[:, c * 128:(c + 1) * 128], identity=idf)
                    return ins
                k.op("pe", tr, reads=[Bsrc, Bcst], writes=[Bpb])
                for j in range(4):
                    c = half * 4 + j
                    gc = colv[:, l * 16 + c:l * 16 + c + 1]
                    bc = colv[:, l * 16 + 8 + c:l * 16 + 8 + c + 1]
                    if half == 0:
                        k.op("act", lambda: A.activation(out=hT[:, c, tok], in_=pb[:, j * 128:(j + 1) * 128], func=AF.Identity,
                                                         scale=gc, bias=bc), reads=[Bpb, Bcol], writes=[BhT[g][0]])
                    else:
                        k.op("dve", lambda: V.tensor_scalar(out=hT[:, c, tok], in0=pb[:, j * 128:(j + 1) * 128], scalar1=gc, scalar2=bc,
                                                           op0=ALU.mult, op1=ALU.add), reads=[Bpb, Bcol], writes=[BhT[g][1]])
                    if router is not None:
                        hf, Bhf, lgp, Blg = router
                        k.op("dve" if j % 2 == 0 else "act",
                             (lambda: V.tensor_scalar(out=hf[:, c, :], in0=pb[:, j * 128:(j + 1) * 128], scalar1=gc, scalar2=bc,
                                                      op0=ALU.mult, op1=ALU.add)) if j % 2 == 0 else
                             (lambda: A.activation(out=hf[:, c, :], in_=pb[:, j * 128:(j + 1) * 128], func=AF.Identity, scale=gc, bias=bc)),
                             reads=[Bpb, Bcol], writes=[Bhf])

        def dump_and_stop(what):
            k.barrier()
            k.op("sp", lambda: SPQ.dma_start(out=dbg2, in_=hT[:].rearrange("p c s -> p (c s)")), dma=dout)
            k.op("sp", lambda: SPQ.dma_start(out=dbg, in_=arena[:]), dma=dout)

        xr = [scr[:, j * 1024:(j + 1) * 1024] for j in range(4)]
        Bxr = [Buf() for _ in range(4)]

        def load_ln_in(b, i, xnt, Bxnt, nring=2):
            s = i % nring
            k.op("sp", lambda: SPQ.dma_start(out=xr[s], in_=x[b, i * 128:(i + 1) * 128, :]), writes=[Bxr[s]], dma=dxr[s])
            rstd, nmr, B = ln_stats(xr[s], Bxr[s])
            k.op("act", lambda: A.activation(out=xnt, in_=xr[s], func=AF.Identity, scale=rstd, bias=nmr),
                 reads=[Bxr[s], B], writes=[Bxnt])

        k.barrier()
        if stop is not None:
            for i in range(NT):
                k.op("dve", lambda: V.memset(arena[:, i * 1024:(i + 1) * 1024], 0.0), writes=[Bxn[i]])
            k.barrier()
        for b in range(NB):
            Wq = wload(0, w_in[:, 0:512], 8)
            Wf = wload(1, w_in[:, 512:1024], 8)
            Wi = wload(2, w_in[:, 1024:1536], 8)
            Wg = wload(3, w_in[:, 1536:2048], 8)
            Wqs = wload(4, w_in[:, 2048:2560], 8)
            Wks = wload(5, w_in[:, 2560:3072], 8)
            xnA = [scr[:, (4 + j) * 1024:(5 + j) * 1024] for j in range(4)]
            BxnA = [Buf() for _ in range(4)]
            for i in range(NT):
                s = i % 4
                load_ln_in(b, i, xnA[s], BxnA[s], nring=4)
                if i >= 1:
                    transpose_evac(xnA[(i - 1) % 4], BxnA[(i - 1) % 4], 0, i - 1)
            transpose_evac(xnA[(NT - 1) % 4], BxnA[(NT - 1) % 4], 0, NT - 1)
            k.barrier()
            if stop == "A":
                dump_and_stop("hT")
                break

            o = 0

            def carve(nbytes, dt, shape=None):
                nonlocal o
                if dt == F32:
                    v = scr[:, o // 4:(o + nbytes) // 4]
                else:
                    v = scrb[:, o // 2:(o + nbytes) // 2]
                o += nbytes
                return v
            Vtok = carve(4096, BF16).rearrange("p (t n) -> p t n", t=4)
            gate2 = carve(8192, F32).rearrange("p (t n) -> p t n", t=4)
            BVtok, Bgate = Buf(), Buf()
            Sst = carve(4096, F32).rearrange("p (h t n) -> p h t n", h=4, t=2)
            BS = [[Buf(), Buf()] for _ in range(4)]
            FR = dict(e=carve(2048, F32), kk=carve(2048, F32), bc=carve(2048, F32), eb=carve(2048, F32))
            FB = {nm: Buf() for nm in ("e", "kk", "bc", "eb")}
            sgt, Bsg = FR["kk"], FB["kk"]
            BK = []
            for r in range(2):
                d = dict(
                    qe=carve(1024, BF16), qlo=carve(1024, BF16), qhi=carve(1024, BF16), keb=carve(1024, BF16),
                    ketok=carve(1024, BF16).rearrange("p (t n) -> p t n", t=4),
                    ketok2=carve(1024, BF16).rearrange("p (t n) -> p t n", t=4),
                    Sbf=carve(2048, BF16).rearrange("p (c n) -> p c n", c=8),
                    ebl=carve(32, F32),
                )
                d["B"] = {nm: Buf() for nm in ("qe", "qlo", "qhi", "keb", "ketok", "Sbf", "ebl")}
                BK.append(d)
            oring = []
            for r in range(3):
                d = dict(AT=carve(256, BF16), sq=carve(512, F32), of=carve(512, F32), ss=carve(16, F32))
                d["B"] = {nm: Buf() for nm in ("AT", "sq", "of", "ss")}
                oring.append(d)
            assert o <= SCR, o
            k.op("dve", lambda: V.memset(Sst.rearrange("p h t n -> p (h t n)"), 0.0), writes=[bb for pr in BS for bb in pr])
            occ = [0]

            def VG(Gi):
                for ti in range(4):
                    i = Gi * 4 + ti
                    tok = slice(i * 128, (i + 1) * 128)
                    pv, Bpv = nextP()

                    def mmv(pp, W):
                        for kc in range(8):
                            ins = T.matmul(pp[:], lhsT=hT[:, kc, tok], rhs=W[:, kc, :], start=(kc == 0), stop=(kc == 7))
                        return ins
                    k.op("pe", lambda: mmv(pv, Wi), reads=[BhT[Gi], Bw[2]], writes=[Bpv])
                    k.op("act", lambda: A.copy(out=Vtok[:, ti, :], in_=pv[:]), reads=[Bpv], writes=[BVtok])
                    pg, Bpg = nextP()
                    k.op("pe", lambda: mmv(pg, Wg), reads=[BhT[Gi], Bw[3]], writes=[Bpg])
                    k.op("act", lambda: A.activation(out=sgt, in_=pg[:], func=AF.Silu), reads=[Bpg], writes=[Bsg])
                    k.op("dve", lambda: V.tensor_tensor(out=gate2[:, ti, :], in0=sgt, in1=ngbc4[:], op=ALU.mult),
                         reads=[Bsg, Bng], writes=[Bgate])

            def front(Gi, h, Kb):
                KB = Kb["B"]
                tokG = slice(Gi * 512, (Gi + 1) * 512)
                hs = slice(h * 128, (h + 1) * 128)
                pq, Bpq = nextP()

                def mmf(pp, W):
                    for kc in range(8):
                        ins = T.matmul(pp[:], lhsT=W[:, kc, hs], rhs=hT[:, kc, tokG], start=(kc == 0), stop=(kc == 7))
                    return ins
                k.op("pe", lambda: mmf(pq, Wq), reads=[BhT[Gi], Bw[0]], writes=[Bpq])
                yield
                pf, Bpf = nextP()
                k.op("pe", lambda: mmf(pf, Wf), reads=[BhT[Gi], Bw[1]], writes=[Bpf])
                yield
                e, kk, bc, eb = FR["e"], FR["kk"], FR["bc"], FR["eb"]
                k.op("act", lambda: A.activation(out=e, in_=pf[:], func=AF.Exp), reads=[Bpf], writes=[FB["e"]])
                yield
                k.op("act", lambda: A.activation(out=e, in_=e, func=AF.Ln, bias=1.0, scale=1.0), reads=[FB["e"], Beps], writes=[FB["e"]])
                yield
                k.op("act", lambda: A.activation(out=e, in_=e, func=AF.Exp, scale=-1.0), reads=[FB["e"]], writes=[FB["e"]])
                yield
                k.op("dve", lambda: V.tensor_scalar(out=kk, in0=e, scalar1=oml[:, h:h + 1], scalar2=None, op0=ALU.mult),
                     reads=[FB["e"], Bcol], writes=[FB["kk"]])
                yield
                k.op("act", lambda: A.activation(out=e, in_=kk, func=AF.Ln, scale=-1.0, bias=1.0),
                     reads=[FB["kk"], Beps], writes=[FB["e"]])
                yield
                k.op("dve", lambda: V.tensor_tensor_scan(out=bc, data0=resetm, data1=e, initial=0.0,
                                                        op0=ALU.mult, op1=ALU.add), reads=[FB["e"], Bcst], writes=[FB["bc"]])
                yield
                k.op("act", lambda: A.activation(out=eb, in_=bc, func=AF.Exp), reads=[FB["bc"]], writes=[FB["eb"]])
                yield
                k.op("act", lambda: A.activation(out=e, in_=bc, func=AF.Exp, scale=-1.0), reads=[FB["bc"]], writes=[FB["e"]])
                yield
                k.op("dve", lambda: V.tensor_tensor(out=Kb["qe"], in0=pq[:], in1=eb, op=ALU.mult), reads=[Bpq, FB["eb"]], writes=[KB["qe"]])
                yield
                k.op("dve", lambda: V.tensor_copy(out=Kb["ebl"], in_=eb.rearrange("p (c t) -> p c t", t=64)[:, :, 63]),
                     reads=[FB["eb"]], writes=[KB["ebl"]])
                yield
                k.op("dve", lambda: V.tensor_tensor(out=e, in0=kk, in1=e, op=ALU.mult), reads=[FB["kk"], FB["e"]], writes=[FB["e"]])
                yield
                k.op("dve", lambda: V.tensor_tensor(out=Kb["qlo"], in0=Kb["qe"], in1=evenm, op=ALU.mult), reads=[KB["qe"], Bcbf], writes=[KB["qlo"]])
                yield
                k.op("dve", lambda: V.tensor_tensor(out=Kb["qhi"], in0=Kb["qe"], in1=oddm, op=ALU.mult), reads=[KB["qe"], Bcbf], writes=[KB["qhi"]])
                yield
                k.op("act", lambda: A.copy(out=Kb["keb"], in_=e), reads=[FB["e"]], writes=[KB["keb"]])
                yield
                pt, Bpt = nextP()

                def trk():
                    for j in range(4):
                        ins = T.transpose(out=pt[:, j * 128:(j + 1) * 128], in_=e[:, j * 128:(j + 1) * 128], identity=idf)
                    return ins
                k.op("pe", trk, reads=[FB["e"], Bcst], writes=[Bpt])
                yield
                k.op("act", lambda: A.activation(out=Kb["ketok"].rearrange("p t n -> p (t n)"), in_=pt[:], func=AF.Identity, scale=mlo),
                     reads=[Bpt, Bcst], writes=[KB["ketok"]])
                yield
                k.op("dve", lambda: V.tensor_scalar(out=Kb["ketok2"].rearrange("p t n -> p (t n)"), in0=pt[:], scalar1=mhi, scalar2=None, op0=ALU.mult),
                     reads=[Bpt, Bcst], writes=[KB["ketok"]])
                yield

            def back(Gi, h, Kb):
                KB = Kb["B"]
                hs = slice(h * 128, (h + 1) * 128)
                pm = [nextP(), nextP()]
                for hf_ in range(2):
                    def mmm():
                        for cc in range(4):
                            c = hf_ * 4 + cc
                            j = c // 2
                            kt = Kb["ketok"] if c % 2 == 0 else Kb["ketok2"]
                            ins = T.matmul(pm[hf_][0][:, cc * 128:(cc + 1) * 128], lhsT=kt[:, j, :], rhs=Vtok[:, j, hs],
                                           start=True, stop=True)
                        return ins
                    k.op("pe", mmm, reads=[KB["ketok"], BVtok], writes=[pm[hf_][1]])
                    yield
                for c in range(8):
                    pmc = pm[c // 4][0][:, (c % 4) * 128:(c % 4 + 1) * 128]
                    k.op("act", lambda: A.activation(out=pmc, in_=pmc, func=AF.Identity, scale=Kb["ebl"][:, c:c + 1]),
                         reads=[pm[c // 4][1], KB["ebl"]], writes=[pm[c // 4][1]])
                    yield
                k.op("act", lambda: A.copy(out=Kb["Sbf"][:, 0, :], in_=Sst[:, h, 0, :]), reads=[BS[h][0]], writes=[KB["Sbf"]])
                yield
                for c in range(8):
                    pmc = pm[c // 4][0][:, (c % 4) * 128:(c % 4 + 1) * 128]
                    src, dst = Sst[:, h, c % 2, :], Sst[:, h, (c + 1) % 2, :]
                    k.op("dve", lambda: V.scalar_tensor_tensor(out=dst, in0=src, scalar=Kb["ebl"][:, c:c + 1], in1=pmc, op0=ALU.mult, op1=ALU.add),
                         reads=[BS[h][c % 2], pm[c // 4][1], KB["ebl"]], writes=[BS[h][(c + 1) % 2]])
                    yield
                    if c < 7:
                        k.op("act", lambda: A.copy(out=Kb["Sbf"][:, c + 1, :], in_=dst), reads=[BS[h][(c + 1) % 2]], writes=[KB["Sbf"]])
                        yield
                st = {}

                def X(j):
                    O = oring[occ[0] % 3]
                    occ[0] += 1
                    OB = O["B"]
                    js = slice(j * 128, (j + 1) * 128)
                    pa, Bpa = nextP()
                    k.op("pe", lambda: T.matmul(pa[:, 0:128], lhsT=Kb["keb"][:, js], rhs=Kb["qe"][:, js], start=True, stop=True),
                         reads=[KB["keb"], KB["qe"]], writes=[Bpa])
                    yield
                    k.op("dve", lambda: V.tensor_tensor(out=O["AT"], in0=pa[:, 0:128], in1=hmask, op=ALU.mult), reads=[Bpa, Bcst], writes=[OB["AT"]])
                    yield
                    po, Bpo = nextP()

                    def mmo():
                        T.matmul(po[:, 0:128], lhsT=O["AT"], rhs=Vtok[:, j, hs], start=True, stop=False)
                        T.matmul(po[:, 0:128], lhsT=Kb["qlo"][:, js], rhs=Kb["Sbf"][:, 2 * j, :], start=False, stop=False)
                        return T.matmul(po[:, 0:128], lhsT=Kb["qhi"][:, js], rhs=Kb["Sbf"][:, 2 * j + 1, :], start=False, stop=True)
                    k.op("pe", mmo, reads=[OB["AT"], BVtok, KB["qlo"], KB["qhi"], KB["Sbf"]], writes=[Bpo])
                    yield
                    st[j] = (O, po, Bpo)

                def Y(j):
                    O, po, Bpo = st[j]
                    OB = O["B"]
                    hs_ = hs
                    k.op("act", lambda: A.activation(out=O["sq"], in_=po[:, 0:128], func=AF.Square), reads=[Bpo], writes=[OB["sq"]])
                    yield
                    k.op("dve", lambda: V.reduce_sum(out=O["ss"][:, 0:1], in_=O["sq"], axis=AX.X), reads=[OB["sq"]], writes=[OB["ss"]])
                    yield
                    k.op("act", lambda: A.activation(out=O["ss"][:, 1:2], in_=O["ss"][:, 0:1], func=AF.Ln, scale=1.0 / 128.0, bias=epsc[:, 1:2]),
                         reads=[OB["ss"], Beps], writes=[OB["ss"]])
                    yield
                    k.op("act", lambda: A.activation(out=O["ss"][:, 2:3], in_=O["ss"][:, 1:2], func=AF.Exp, scale=-0.5), reads=[OB["ss"]], writes=[OB["ss"]])
                    yield
                    k.op("dve", lambda: V.scalar_tensor_tensor(out=O["of"], in0=po[:, 0:128], scalar=O["ss"][:, 2:3], in1=gate2[:, j, hs_],
                                                              op0=ALU.mult, op1=ALU.mult), reads=[Bpo, OB["ss"], Bgate], writes=[OB["of"]])
                    yield

                def Z(j):
                    O, po, Bpo = st[j]
                    OB = O["B"]
                    p2, Bp2 = nextP()
                    k.op("pe", lambda: T.transpose(out=p2[:, 0:128], in_=O["of"], identity=idf), reads=[OB["of"], Bcst], writes=[Bp2])
                    yield
                    k.op("act", lambda: A.copy(out=oaT[:, h, Gi * 512 + j * 128:Gi * 512 + (j + 1) * 128], in_=p2[:, 0:128]),
                         reads=[Bp2], writes=[Boa[Gi]])
                    yield
                for fn_, j_ in ((X, 0), (X, 1), (Y, 0), (X, 2), (Y, 1), (Z, 0), (X, 3), (Y, 2), (Z, 1), (Y, 3), (Z, 2), (Z, 3)):
                    yield from fn_(j_)

            def run_il(*gens):
                gens = list(gens)
                while gens:
                    for g_ in list(gens):
                        try:
                            next(g_)
                        except StopIteration:
                            gens.remove(g_)
            def run_w(main, side, ratio):
                n = 0
                alive = True
                for _ in main:
                    n += 1
                    if alive and n % ratio == 0:
                        try:
                            next(side)
                        except StopIteration:
                            alive = False
                if alive:
                    for _ in side:
                        pass
            RW = int(os.environ.get("KRW", "4"))
            itb = 0
            VG(0)
            run_il(front(0, 0, BK[0]))
            for Gi in range(NG):
                for h in range(4):
                    cur = BK[itb % 2]
                    if h + 1 < 4:
                        run_w(back(Gi, h, cur), front(Gi, h + 1, BK[(itb + 1) % 2]), RW)
                    elif Gi + 1 < NG:
                        run_w(back(Gi, h, cur), front(Gi + 1, 0, BK[(itb + 1) % 2]), RW)
                        VG(Gi + 1)
                    else:
                        run_il(back(Gi, h, cur))
                    itb += 1
            k.barrier()
            if stop == "B":
                dump_and_stop("arena")
                break

            Wvs = wload(0, w_in[:, 3072:3584], 8)
            Wa = wload(1, wa_d, 4)
            Wb = wload(2, wb_d, 4)
            Wga0 = wload(3, w_in[:, 3584:4096], 8)
            o = 0
            qTp = carve(2 * S, BF16)
            kTp = [carve(2 * S, BF16), carve(2 * S, BF16)]
            Vpad = carve(NT * 512, BF16).rearrange("p (i u n) -> p i u n", i=NT, u=2)
            BqT, BkT, BVp = Buf(), Buf(), (Buf(), Buf())
            cr = []
            for u in range(2):
                for par in range(2):
                    d = dict(e=carve(2048, F32), sp=carve(1024, BF16), et=carve(2048, F32), WT=carve(1024, BF16))
                    d["B"] = {nm: Buf() for nm in ("e", "sp", "et", "WT")}
                    cr.append(d)
            assert o <= SCR, o
            k.op("pool", lambda: G_.memset(Vpad.rearrange("p i u n -> p (i u n)"), 0.0), writes=[BVp])
            for hp in range(4):
                hps = slice(hp * 128, (hp + 1) * 128)
                for Gi in range(NG):
                    tokG = slice(Gi * 512, (Gi + 1) * 512)
                    for (W, wi, dst, Bd, eng) in ((Wqs, 4, qTp, BqT, "act"), (Wks, 5, kTp, BkT, "dve")):
                        pp, Bpp = nextP()

                        def mmp(pp=pp, W=W):
                            for kc in range(8):
                                ins = T.matmul(pp[:], lhsT=W[:, kc, hps], rhs=hT[:, kc, tokG], start=(kc == 0), stop=(kc == 7))
                            return ins
                        k.op("pe", mmp, reads=[BhT[Gi], Bw[wi]], writes=[Bpp])
                        if eng == "act":
                            k.op("act", lambda: A.copy(out=dst[:, tokG], in_=pp[:]), reads=[Bpp], writes=[Bd])
                        else:
                            k.op("dve", lambda: V.tensor_scalar(out=dst[0][:, tokG], in0=pp[:], scalar1=mlo, scalar2=None, op0=ALU.mult),
                                 reads=[Bpp, Bcst], writes=[Bd])
                            k.op("dve", lambda: V.tensor_scalar(out=dst[1][:, tokG], in0=pp[:], scalar1=mhi, scalar2=None, op0=ALU.mult),
                                 reads=[Bpp, Bcst], writes=[Bd])
                for i in range(NT):
                    tok = slice(i * 128, (i + 1) * 128)
                    pp, Bpp = nextP()

                    def mmv2():
                        for kc in range(8):
                            ins = T.matmul(pp[:, 0:128], lhsT=hT[:, kc, tok], rhs=Wvs[:, kc, hps], start=(kc == 0), stop=(kc == 7))
                        return ins
                    k.op("pe", mmv2, reads=[BhT[i // 4], Bw[0]], writes=[Bpp])
                    if i % 2 == 0:
                        k.op("act", lambda: A.copy(out=Vpad[:, i, 0, 0:64], in_=pp[:, 0:64]), reads=[Bpp], writes=[BVp[0]])
                        k.op("act", lambda: A.copy(out=Vpad[:, i, 1, 64:128], in_=pp[:, 64:128]), reads=[Bpp], writes=[BVp[0]])
                    else:
                        k.op("dve", lambda: V.tensor_copy(out=Vpad[:, i, 0, 0:64], in_=pp[:, 0:64]), reads=[Bpp], writes=[BVp[1]])
                        k.op("dve", lambda: V.tensor_copy(out=Vpad[:, i, 1, 64:128], in_=pp[:, 64:128]), reads=[Bpp], writes=[BVp[1]])
                Z = [(P[0], BP[0]), (P[1], BP[1]), (P[2], BP[2]), (P[3], BP[3])]
                Tb = [(P[4], BP[4]), (P[5], BP[5])]
                oTb = (P[6], BP[6])
                for qc in range(NG):
                    qs0 = qc * 512
                    for u in range(2):
                        k.op("pe", lambda: T.matmul(Tb[u][0][:], lhsT=zeros_bf, rhs=qTp[:, qs0:qs0 + 512], start=True, stop=True),
                             reads=[Bcbf, BqT], writes=[Tb[u][1]])
                    k.op("pe", lambda: T.matmul(oTb[0][:], lhsT=zeros_bf, rhs=qTp[:, qs0:qs0 + 512], start=True, stop=True),
                         reads=[Bcbf, BqT], writes=[oTb[1]])
                    steps = list(range(qc * 4 + 3, -1, -1))

                    def zmm(si):
                        kb = steps[si]
                        c0 = max(0, kb - qc * 4) * 128
                        w = 512 - c0
                        for u in range(2):
                            zt, Bz = Z[(si % 2) * 2 + u]
                            def zz():
                                ins = T.matmul(zt[:, 0:w], lhsT=kTp[u][:, kb * 128:(kb + 1) * 128], rhs=qTp[:, qs0 + c0:qs0 + 512],
                                               start=True, stop=True)
                                if kb >= qc * 4:
                                    ins = T.matmul(zt[:, 0:128], lhsT=ident_bf, rhs=neg_bf, start=False, stop=True, skip_group_check=True)
                                return ins
                            k.op("pe", zz, reads=[BkT, BqT, Bcbf], writes=[Bz])
                    def geom(si):
                        kb = steps[si]
                        c0 = max(0, kb - qc * 4) * 128
                        return kb, c0, 512 - c0, kb >= qc * 4

                    def stA(si):
                        kb, c0, w, diag = geom(si)
                        for u in range(2):
                            zt, Bz = Z[(si % 2) * 2 + u]
                            C = cr[u * 2 + si % 2]
                            CB = C["B"]
                            k.op("act", lambda: A.activation(out=C["e"][:, 0:w], in_=zt[:, 0:w], func=AF.Exp, scale=0.125), reads=[Bz], writes=[CB["e"]])
                            k.op("act", lambda: A.activation(out=C["sp"][:, 0:w], in_=C["e"][:, 0:w], func=AF.Ln, bias=1.0, scale=1.0),
                                 reads=[CB["e"], Beps], writes=[CB["sp"]])

                    def stL(si):
                        kb, c0, w, diag = geom(si)
                        for u in range(2):
                            C = cr[u * 2 + si % 2]
                            CB = C["B"]
                            k.op("pe", lambda: T.matmul(Tb[u][0][:, c0:512], lhsT=L_bf, rhs=C["sp"][:, 0:w], start=False, stop=True, skip_group_check=True),
                                 reads=[Bcbf, CB["sp"]], writes=[Tb[u][1]])

                    def stTail(si):
                        kb, c0, w, diag = geom(si)
                        for u in range(2):
                            C = cr[u * 2 + si % 2]
                            CB = C["B"]
                            k.op("act", lambda: A.activation(out=C["et"][:, 0:w], in_=Tb[u][0][:, c0:512], func=AF.Exp, scale=-1.0),
                                 reads=[Tb[u][1]], writes=[CB["et"]])
                            k.op("dve", lambda: V.tensor_tensor(out=C["WT"][:, 0:w], in0=C["e"][:, 0:w], in1=C["et"][:, 0:w], op=ALU.mult),
                                 reads=[CB["e"], CB["et"]], writes=[CB["WT"]])
                        for u in range(2):
                            C = cr[u * 2 + si % 2]
                            CB = C["B"]
                            k.op("pe", lambda: T.matmul(Tb[u][0][:, c0:512], lhsT=U_bf, rhs=C["sp"][:, 0:w], start=False, stop=True, skip_group_check=True),
                                 reads=[Bcbf, CB["sp"]], writes=[Tb[u][1]])
                            k.op("pe", lambda: T.matmul(oTb[0][:, c0:512], lhsT=Vpad[:, kb, u, :], rhs=C["WT"][:, 0:w], start=False, stop=True, skip_group_check=True),
                                 reads=[BVp, CB["WT"]], writes=[oTb[1]])
                    zmm(0)
                    for si in range(len(steps)):
                        stA(si)
                        if si + 1 < len(steps):
                            zmm(si + 1)
                        stL(si)
                        stTail(si)
                    k.op("act", lambda: A.copy(out=obT[:, hp, qs0:qs0 + 512], in_=oTb[0][:]), reads=[oTb[1]], writes=[Bob[qc]])
            k.barrier()
            if stop == "C":
                dump_and_stop("arena")
                break

            Wgb0 = wload(4, w_in[:, 4608:5120], 8)
            Wga1 = wload(5, w_in[:, 4096:4608], 8)
            Wgb1 = wload(0, w_in[:, 5120:5632], 8)
            o = 0
            dr = []
            for r in range(2):
                d = dict(sga=carve(2048, F32), sgb=carve(2048, F32), t1=carve(2048, F32), t2=carve(2048, F32))
                d["B"] = {nm: Buf() for nm in ("sga", "sgb", "t1", "t2")}
                dr.append(d)
            assert o <= SCR
            dc_ = 0
            Wmx = [None, None]
            for q in range(2):
                Wga, iga = (Wga0, 3) if q == 0 else (Wga1, 5)
                Wgb, igb = (Wgb0, 4) if q == 0 else (Wgb1, 0)
                for Gi in range(NG):
                    tokG = slice(Gi * 512, (Gi + 1) * 512)
                    for dc in range(4):
                        Rr = dr[dc_ % 2]
                        RB = Rr["B"]
                        dc_ += 1
                        cs = slice(dc * 128, (dc + 1) * 128)
                        cs2 = slice(q * 512 + dc * 128, q * 512 + (dc + 1) * 128)
                        pga, Bpga = nextP()
                        pgb, Bpgb = nextP()
                        pya, Bpya = nextP()
                        pyb, Bpyb = nextP()

                        def mm8(pp, W, csl):
                            for kc in range(8):
                                ins = T.matmul(pp[:], lhsT=W[:, kc, csl], rhs=hT[:, kc, tokG], start=(kc == 0), stop=(kc == 7))
                            return ins

                        def mm4(pp, W, src):
                            for kc in range(4):
                                ins = T.matmul(pp[:], lhsT=W[:, kc, cs2], rhs=src[:, kc, tokG], start=(kc == 0), stop=(kc == 3))
                            return ins
                        k.op("pe", lambda: mm8(pga, Wga, cs), reads=[BhT[Gi], Bw[iga]], writes=[Bpga])
                        k.op("pe", lambda: mm8(pgb, Wgb, cs), reads=[BhT[Gi], Bw[igb]], writes=[Bpgb])
                        k.op("pe", lambda: mm4(pya, Wa, oaT), reads=[Boa[Gi], Bw[1]], writes=[Bpya])
                        k.op("pe", lambda: mm4(pyb, Wb, obT), reads=[Bob[Gi], Bw[2]], writes=[Bpyb])
                        k.op("act", lambda: A.activation(out=Rr["sga"], in_=pga[:], func=AF.Sigmoid), reads=[Bpga], writes=[RB["sga"]])
                        k.op("act", lambda: A.activation(out=Rr["sgb"], in_=pgb[:], func=AF.Sigmoid), reads=[Bpgb], writes=[RB["sgb"]])
                        k.op("dve", lambda: V.tensor_tensor(out=Rr["t1"], in0=Rr["sga"], in1=pya[:], op=ALU.mult), reads=[RB["sga"], Bpya], writes=[RB["t1"]])
                        k.op("dve", lambda: V.tensor_tensor(out=Rr["t2"], in0=Rr["sgb"], in1=pyb[:], op=ALU.mult), reads=[RB["sgb"], Bpyb], writes=[RB["t2"]])
                        k.op("dve", lambda: V.tensor_tensor(out=mrg[:, Gi * 4:(Gi + 1) * 4, q * 4 + dc, :],
                                                              in0=Rr["t1"].rearrange("p (t n) -> p t n", t=4),
                                                              in1=Rr["t2"].rearrange("p (t n) -> p t n", t=4), op=ALU.add),
                             reads=[RB["t1"], RB["t2"]], writes=[Bmrg[Gi * 4 + tt] for tt in range(4)])
                if q == 0:
                    Wmx[0] = wload(3, wmix_d[:, 0:512], 8)
                    Wmx[1] = wload(4, wmix_d[:, 512:1024], 8)
            k.barrier()
            if stop == "D1":
                dump_and_stop("arena")
                break

            def resid_a(i, xprev, Bxprev, lhs_fn, lhs_reads, Wn, Wn_slots, nk, ut, But):
                banks = [nextP(), nextP()]
                for n in range(2):
                    pp, Bpp = banks[n]

                    def mmr():
                        for kc in range(nk):
                            ins = T.matmul(pp[:], lhsT=lhs_fn(kc), rhs=Wn[n][:, kc, :], start=(kc == 0), stop=(kc == nk - 1))
                        return ins
                    k.op("pe", mmr, reads=list(lhs_reads) + [Bw[Wn_slots[n]]], writes=[Bpp])
                k.op("pool", lambda: G_.tensor_tensor(out=ut, in0=xprev, in1=agbc[:], op=ALU.mult), reads=[Bxprev, Bagbc], writes=[But])
                k.op("pool", lambda: G_.tensor_tensor(out=ut, in0=ut, in1=abbc[:], op=ALU.add), reads=[But, Babbc], writes=[But])
                return banks

            def resid_b(i, banks, ut, But):
                for n in range(2):
                    pp, Bpp = banks[n]
                    ns = slice(n * 512, (n + 1) * 512)
                    k.op("dve", lambda: V.tensor_tensor(out=ut[:, ns], in0=ut[:, ns], in1=pp[:], op=ALU.add), reads=[But, Bpp], writes=[But])
                rstd, nmr, B = ln_stats(ut, But)
                xo = arena[:, i * 1024:(i + 1) * 1024]
                k.op("act", lambda: A.activation(out=xo, in_=ut, func=AF.Identity, scale=rstd, bias=nmr), reads=[But, B], writes=[Bxn[i]])

            load_resid_consts(0)
            Wk_ = [wload(5, wk_d[:, 0:512], 8), wload(0, wk_d[:, 512:1024], 8)]
            Wv_ = [wload(1, wv_d[:, 0:512], 8), wload(2, wv_d[:, 512:1024], 8)]
            utl = [scr[:, (8 + j) * 1024:(9 + j) * 1024] for j in range(3)]
            Butl = [Buf(), Buf(), Buf()]
            def d2_a(i):
                s = i % 3
                return resid_a(i, xnA[s], BxnA[s], lambda kc: mrg[:, i, kc, :], [Bmrg[i]], Wmx, (3, 4), 8, utl[s], Butl[s])
            load_ln_in(b, 0, xnA[0], BxnA[0], nring=3)
            if NT > 1:
                load_ln_in(b, 1, xnA[1], BxnA[1], nring=3)
            bk = {0: d2_a(0)}
            for i in range(NT):
                if i + 2 < NT:
                    load_ln_in(b, i + 2, xnA[(i + 2) % 3], BxnA[(i + 2) % 3], nring=3)
                if i + 1 < NT:
                    bk[i + 1] = d2_a(i + 1)
                resid_b(i, bk.pop(i), utl[i % 3], Butl[i % 3])
                if i >= 1:
                    transpose_evac(arena[:, (i - 1) * 1024:i * 1024], Bxn[i - 1], 1, i - 1)
            transpose_evac(arena[:, (NT - 1) * 1024:NT * 1024], Bxn[NT - 1], 1, NT - 1)
            k.barrier()
            if stop == "D2":
                dump_and_stop("arena")
                break

            load_resid_consts(1)
            Wq_ = [wload(3, wq_d[:, 0:512], 8), wload(4, wq_d[:, 512:1024], 8)]
            o = 8192
            hf = carve(8 * 128 * 4, F32).rearrange("p (c t) -> p c t", c=8)
            memT = hf.rearrange("p c t -> p (c t)").bitcast(BF16)[:, 0:2048].rearrange("p (c m) -> p c m", c=8)
            kTm = carve(8 * 256 * 2, BF16).rearrange("p (c m) -> p c m", c=8)
            vm = carve(2 * 1024 * 2, BF16).rearrange("p (t n) -> p t n", t=2)
            qTg = carve(8 * 512 * 2, BF16).rearrange("p (c t) -> p c t", c=8)
            oTg = carve(8 * 512 * 2, BF16).rearrange("p (c t) -> p c t", c=8)
            pTm = [carve(1024, BF16) for _ in range(4)]
            rinv = carve(2048, F32)
            rtb = [carve(448, F32) for _ in range(4)]
            Brtb = [Buf() for _ in range(4)]
            Blgb = [Buf() for _ in range(4)]
            assert o <= SCR, o
            utl = [scr[:, 0:1024], scr[:, 1024:2048]]
            BmemT, BkTm, Bvm, BqTg, BoTg, Brinv, Bhf = [Buf() for _ in range(7)]
            rinvs = [(rinv, Brinv), (hf.rearrange("p c t -> p (c t)")[:, 0:512], Buf())]
            BpTm = [Buf() for _ in range(4)]
            for mt in range(2):
                k.op("sp", lambda: SPQ.dma_start(out=xr[mt], in_=mem[b, mt * 128:(mt + 1) * 128, :]), writes=[Bxr[mt]], dma=dxr[mt])
                for half in range(2):
                    pb, Bpb = nextP()

                    def trm():
                        for j in range(4):
                            c = half * 4 + j
                            ins = T.transpose(out=pb[:, j * 128:(j + 1) * 128], in_=xr[mt][:, c * 128:(c + 1) * 128], identity=idf)
                        return ins
                    k.op("pe", trm, reads=[Bxr[mt], Bcst], writes=[Bpb])
                    k.op("act", lambda: A.copy(out=memT[:, half * 4:(half + 1) * 4, mt * 128:(mt + 1) * 128],
                                               in_=pb[:].rearrange("p (j t) -> p j t", j=4)), reads=[Bpb], writes=[BmemT])
            for c in range(8):
                pp, Bpp = nextP()
                Wkk = Wk_[c // 4]
                cs = slice((c % 4) * 128, (c % 4 + 1) * 128)

                def mmk():
                    for kc in range(8):
                        ins = T.matmul(pp[:, 0:256], lhsT=Wkk[:, kc, cs], rhs=memT[:, kc, :], start=(kc == 0), stop=(kc == 7))
                    return ins
                k.op("pe", mmk, reads=[BmemT, Bw[5 if c < 4 else 0]], writes=[Bpp])
                k.op("act" if c % 2 == 0 else "dve",
                     (lambda: A.copy(out=kTm[:, c, :], in_=pp[:, 0:256])) if c % 2 == 0 else (lambda: V.tensor_copy(out=kTm[:, c, :], in_=pp[:, 0:256])),
                     reads=[Bpp], writes=[BkTm])
            for mt in range(2):
                for n in range(2):
                    pp, Bpp = nextP()

                    def mmvv():
                        for kc in range(8):
                            ins = T.matmul(pp[:], lhsT=memT[:, kc, mt * 128:(mt + 1) * 128], rhs=Wv_[n][:, kc, :], start=(kc == 0), stop=(kc == 7))
                        return ins
                    k.op("pe", mmvv, reads=[BmemT, Bw[1 + n]], writes=[Bpp])
                    k.op("act", lambda: A.copy(out=vm[:, mt, n * 512:(n + 1) * 512], in_=pp[:]), reads=[Bpp], writes=[Bvm])
            k.barrier()
            Wo_ = [wload(5, wo_d[:, 0:512], 8), wload(0, wo_d[:, 512:1024], 8)]
            def router_tile(i, Gi, rt, Brt, lgs, Blgs):
                pl, Bpl = nextP()

                def mmrt():
                    for kc in range(8):
                        ins = T.matmul(pl[:, 0:20], lhsT=hT[:, kc, i * 128:(i + 1) * 128], rhs=wrb[:, kc, :], start=(kc == 0), stop=(kc == 7))
                    return ins
                k.op("pe", mmrt, reads=[BhT[Gi], Bwr], writes=[Bpl])
                yield
                k.op("dve", lambda: V.tensor_tensor(out=lgs, in0=pl[:, 0:20], in1=brbc[:], op=ALU.add), reads=[Bpl, Bbrr], writes=[Blgs])
                yield
                R_ = [Blgs]
                k.op("dve", lambda: V.reduce_max(out=rt[:, 0:1], in_=lgs[:, 0:4], axis=AX.X), reads=R_, writes=[Brt])
                yield
                k.op("dve", lambda: V.tensor_scalar(out=rt[:, 1:2], in0=rt[:, 0:1], scalar1=-1.0, scalar2=None, op0=ALU.mult), reads=[Brt], writes=[Brt])
                yield
                k.op("act", lambda: A.activation(out=rt[:, 4:8], in_=lgs[:, 0:4], func=AF.Exp, bias=rt[:, 1:2], scale=1.0), reads=[Brt, Blgs], writes=[Brt])
                yield
                k.op("dve", lambda: V.reduce_sum(out=rt[:, 2:3], in_=rt[:, 4:8], axis=AX.X), reads=[Brt], writes=[Brt])
                yield
                k.op("dve", lambda: V.reciprocal(out=rt[:, 3:4], in_=rt[:, 2:3]), reads=[Brt], writes=[Brt])
                yield
                k.op("dve", lambda: V.tensor_scalar(out=rt[:, 8:12], in0=lgs[:, 0:4], scalar1=rt[:, 0:1], scalar2=None, op0=ALU.is_equal),
                     reads=[Brt, Blgs], writes=[Brt])
                yield
                k.op("dve", lambda: V.tensor_tensor(out=rt[:, 16:32].rearrange("p (k j) -> p k j", k=4),
                                                    in0=lgs[:, 4:20].rearrange("p (j k) -> p k j", j=4),
                                                    in1=rt[:, 8:12].unsqueeze(1).to_broadcast([128, 4, 4]), op=ALU.mult),
                     reads=[Brt, Blgs], writes=[Brt])
                yield
                k.op("dve", lambda: V.tensor_reduce(out=rt[:, 12:16], in_=rt[:, 16:32].rearrange("p (k j) -> p k j", k=4), axis=AX.X, op=ALU.add),
                     reads=[Brt], writes=[Brt])
                yield
                k.op("dve", lambda: V.reduce_max(out=rt[:, 32:33], in_=rt[:, 12:16], axis=AX.X), reads=[Brt], writes=[Brt])
                yield
                k.op("dve", lambda: V.tensor_scalar(out=rt[:, 36:40], in0=rt[:, 12:16], scalar1=rt[:, 32:33], scalar2=None, op0=ALU.is_equal),
                     reads=[Brt], writes=[Brt])
                yield
                k.op("dve", lambda: V.scalar_tensor_tensor(out=rt[:, 40:44], in0=rt[:, 36:40], scalar=-1e30, in1=rt[:, 12:16],
                                                          op0=ALU.mult, op1=ALU.add), reads=[Brt], writes=[Brt])
                yield
                k.op("dve", lambda: V.reduce_max(out=rt[:, 33:34], in_=rt[:, 40:44], axis=AX.X), reads=[Brt], writes=[Brt])
                yield
                k.op("dve", lambda: V.tensor_scalar(out=rt[:, 44:48], in0=rt[:, 40:44], scalar1=rt[:, 33:34], scalar2=None, op0=ALU.is_equal),
                     reads=[Brt], writes=[Brt])
                yield
                k.op("dve", lambda: V.tensor_tensor(out=rt[:, 34:35], in0=rt[:, 33:34], in1=rt[:, 32:33], op=ALU.subtract), reads=[Brt], writes=[Brt])
                yield
                k.op("act", lambda: A.activation(out=rt[:, 35:36], in_=rt[:, 34:35], func=AF.Exp), reads=[Brt], writes=[Brt])
                yield
                k.op("dve", lambda: V.tensor_scalar_add(out=rt[:, 48:49], in0=rt[:, 35:36], scalar1=1.0), reads=[Brt], writes=[Brt])
                yield
                k.op("dve", lambda: V.reciprocal(out=rt[:, 49:50], in_=rt[:, 48:49]), reads=[Brt], writes=[Brt])
                yield
                k.op("dve", lambda: V.tensor_tensor(out=rt[:, 50:51], in0=rt[:, 49:50], in1=rt[:, 3:4], op=ALU.mult), reads=[Brt], writes=[Brt])
                yield
                k.op("dve", lambda: V.tensor_tensor(out=rt[:, 51:52], in0=rt[:, 50:51], in1=rt[:, 35:36], op=ALU.mult), reads=[Brt], writes=[Brt])
                yield
                k.op("dve", lambda: V.tensor_scalar(out=rt[:, 52:56], in0=rt[:, 36:40], scalar1=rt[:, 50:51], scalar2=None, op0=ALU.mult),
                     reads=[Brt], writes=[Brt])
                yield
                k.op("dve", lambda: V.scalar_tensor_tensor(out=rt[:, 52:56], in0=rt[:, 44:48], scalar=rt[:, 51:52], in1=rt[:, 52:56],
                                                          op0=ALU.mult, op1=ALU.add), reads=[Brt], writes=[Brt])
                yield
                k.op("dve", lambda: V.tensor_tensor(out=rt[:, 64:80].rearrange("p (j k) -> p j k", j=4),
                                                    in0=rt[:, 8:12].unsqueeze(2).to_broadcast([128, 4, 4]),
                                                    in1=rt[:, 52:56].unsqueeze(1).to_broadcast([128, 4, 4]), op=ALU.mult),
                     reads=[Brt], writes=[Brt])
                yield
                k.op("dve", lambda: V.tensor_copy(out=cmbA[:, i, :], in_=rt[:, 64:80]), reads=[Brt], writes=[BcmbA[i]])
                yield

            KE = int(os.environ.get("KE", "9"))
            for Gi in range(NG if KE >= 2 else 0):
                tokG = slice(Gi * 512, (Gi + 1) * 512)
                for c in range(8):
                    pp, Bpp = nextP()
                    Wqq = Wq_[c // 4]
                    cs = slice((c % 4) * 128, (c % 4 + 1) * 128)

                    def mmq():
                        for kc in range(8):
                            ins = T.matmul(pp[:], lhsT=Wqq[:, kc, cs], rhs=hT[:, kc, tokG], start=(kc == 0), stop=(kc == 7))
                        return ins
                    k.op("pe", mmq, reads=[BhT[Gi], Bw[3 + c // 4]], writes=[Bpp])
                    k.op("act" if c % 2 == 0 else "dve",
                         (lambda: A.copy(out=qTg[:, c, :], in_=pp[:])) if c % 2 == 0 else (lambda: V.tensor_copy(out=qTg[:, c, :], in_=pp[:])),
                         reads=[Bpp], writes=[BqTg])
                def att_head(h):
                    rv, Brv = rinvs[h % 2]
                    for mt in range(2):
                        pp, Bpp = nextP()
                        ms = slice(mt * 128, (mt + 1) * 128)

                        def mms():
                            T.matmul(pp[:], lhsT=kTm[:, 2 * h, ms], rhs=qTg[:, 2 * h, :], start=True, stop=False)
                            return T.matmul(pp[:], lhsT=kTm[:, 2 * h + 1, ms], rhs=qTg[:, 2 * h + 1, :], start=False, stop=True)
                        k.op("pe", mms, reads=[BkTm, BqTg], writes=[Bpp])
                        yield
                        pi = (h % 2) * 2 + mt
                        k.op("act", lambda: A.activation(out=pTm[pi], in_=pp[:], func=AF.Exp, scale=1.0 / 16.0), reads=[Bpp], writes=[BpTm[pi]])
                        yield
                    psum_, Bps_ = nextP()
                    pA = [pTm[(h % 2) * 2], pTm[(h % 2) * 2 + 1]]
                    BpA = [BpTm[(h % 2) * 2], BpTm[(h % 2) * 2 + 1]]

                    def mmsum():
                        T.matmul(psum_[:], lhsT=ones_bf, rhs=pA[0], start=True, stop=False)
                        return T.matmul(psum_[:], lhsT=ones_bf, rhs=pA[1], start=False, stop=True)
                    k.op("pe", mmsum, reads=BpA + [Bcbf], writes=[Bps_])
                    yield
                    k.op("act", lambda: A.activation(out=rv, in_=psum_[:], func=AF.Ln), reads=[Bps_], writes=[Brv])
                    yield
                    k.op("act", lambda: A.activation(out=rv, in_=rv, func=AF.Exp, scale=-1.0), reads=[Brv], writes=[Brv])
                    yield
                    for jj in range(2):
                        c = 2 * h + jj
                        pp, Bpp = nextP()

                        def mmpv():
                            T.matmul(pp[:], lhsT=vm[:, 0, c * 128:(c + 1) * 128], rhs=pA[0], start=True, stop=False)
                            return T.matmul(pp[:], lhsT=vm[:, 1, c * 128:(c + 1) * 128], rhs=pA[1], start=False, stop=True)
                        k.op("pe", mmpv, reads=BpA + [Bvm], writes=[Bpp])
                        yield
                        k.op("dve", lambda: V.tensor_tensor(out=oTg[:, c, :], in0=pp[:], in1=rv, op=ALU.mult), reads=[Bpp, Brv], writes=[BoTg])
                        yield
                for hh in (0, 2):
                    gens = [att_head(hh), att_head(hh + 1)]
                    while gens:
                        for g_ in list(gens):
                            try:
                                next(g_)
                            except StopIteration:
                                gens.remove(g_)
                def e_a(ti):
                    i = Gi * 4 + ti
                    ts_ = slice(ti * 128, (ti + 1) * 128)
                    return resid_a(i, arena[:, i * 1024:(i + 1) * 1024], Bxn[i], lambda kc: oTg[:, kc, ts_], [BoTg], Wo_, (5, 0), 8,
                                   utl[i % 2], Butl[i % 2])
                bk = {0: e_a(0)}
                for ti in range(4):
                    i = Gi * 4 + ti
                    if ti + 1 < 4:
                        bk[ti + 1] = e_a(ti + 1)
                    resid_b(i, bk.pop(ti), utl[i % 2], Butl[i % 2])
                    if ti >= 1:
                        transpose_evac(arena[:, (i - 1) * 1024:i * 1024], Bxn[i - 1], 2, i - 1)
                iL = Gi * 4 + 3
                transpose_evac(arena[:, iL * 1024:(iL + 1) * 1024], Bxn[iL], 2, iL)
                gens = [router_tile(Gi * 4 + j, Gi, rtb[j][:, 0:80], Brtb[j], rtb[j][:, 80:100], Blgb[j]) for j in range(4)]
                while gens:
                    for g_ in list(gens):
                        try:
                            next(g_)
                        except StopIteration:
                            gens.remove(g_)
            k.barrier()
            if stop == "E":
                dump_and_stop("arena")
                break

            load_resid_consts(2)
            o = 0
            hid = [carve(4096, BF16).rearrange("p (c t) -> p c t", c=4) for _ in range(2)]
            Bhid = [Buf(), Buf()]
            fr = []
            for r in range(2):
                d = dict(sa=carve(2048, F32), t=carve(2048, F32))
                d["B"] = {nm: Buf() for nm in ("sa", "t")}
                fr.append(d)
            cmbs = [carve(2048, F32) for _ in range(2)]
            Bcmbs = [Buf(), Buf()]
            cbc = [carve(512, F32) for _ in range(2)]
            Bcbc = [Buf(), Buf()]
            outr = [carve(4096, F32), carve(4096, F32)]
            Boutr = [Buf(), Buf()]
            g3bc = carve(4096, F32)
            b3bc = carve(4096, F32)
            assert o <= SCR, o
            k.op("sp", lambda: SPQ.dma_start(out=g3bc, in_=rowv_d[3:4, :].partition_broadcast(128)), writes=[Bg3], dma=dg3)
            k.op("sp", lambda: SPQ.dma_start(out=b3bc, in_=rowv_d[4:5, :].partition_broadcast(128)), writes=[Bb3], dma=db3)
            for i in range(NT):
                xo = arena[:, i * 1024:(i + 1) * 1024]
                k.op("dve", lambda: V.tensor_tensor(out=xo, in0=xo, in1=agbc[:], op=ALU.mult), reads=[Bxn[i], Bagbc], writes=[Bxn[i]])
                k.op("dve", lambda: V.tensor_tensor(out=xo, in0=xo, in1=abbc[:], op=ALU.add), reads=[Bxn[i], Babbc], writes=[Bxn[i]])

            def load_expert(e):
                base = (e % 2) * 3
                return (wload(base, w1_d[e], 8), wload(base + 1, w3_d[e], 8), wload(base + 2, w2_d[e], 4), base)
            PA = [(P[0], BP[0]), (P[1], BP[1])]
            PB = [(P[2], BP[2]), (P[3], BP[3])]
            PY = [(P[4], BP[4]), (P[5], BP[5]), (P[6], BP[6])]
            PC = (P[7], BP[7])
            Wcur = load_expert(0)
            fc = 0
            yc = 0
            gc_ = 0
            pend = None

            def emit_y(pd, last=False):
                nonlocal yc
                H, gi2, Gi, W2, base = pd
                for ti in range(4):
                    i = Gi * 4 + ti
                    ts_ = slice(ti * 128, (ti + 1) * 128)
                    xo = arena[:, i * 1024:(i + 1) * 1024]
                    for n in range(2):
                        ns = slice(n * 512, (n + 1) * 512)
                        py, Bpy = PY[yc % 3]
                        yc += 1

                        def mmy():
                            for kc in range(4):
                                ins = T.matmul(py[:], lhsT=H[:, kc, ts_], rhs=W2[:, kc, ns], start=(kc == 0), stop=(kc == 3))
                            return ins
                        k.op("pe", mmy, reads=[Bhid[gi2], Bw[base + 2]], writes=[Bpy])
                        k.op("dve", lambda: V.tensor_tensor(out=xo[:, ns], in0=xo[:, ns], in1=py[:], op=ALU.add), reads=[Bxn[i], Bpy], writes=[Bxn[i]])
                    if last:
                        ln3_group(Gi, only=i)
            cbn = [0]

            def cmb_prep(e_, G_i, g2):
                for ti in range(4):
                    i = G_i * 4 + ti
                    cbi = cbn[0] % 2
                    cbn[0] += 1
                    k.op("act", lambda: A.copy(out=cbc[cbi], in_=cmbA[:, i, e_:e_ + 1].to_broadcast([128, 128])), reads=[BcmbA[i]], writes=[Bcbc[cbi]])
                    k.op("pe", lambda: T.matmul(PC[0][:, ti * 128:(ti + 1) * 128], lhsT=cbc[cbi], rhs=idf, start=True, stop=True),
                         reads=[Bcbc[cbi], Bcst], writes=[PC[1]])
                k.op("act", lambda: A.copy(out=cmbs[g2], in_=PC[0][:]), reads=[PC[1]], writes=[Bcmbs[g2]])

            def ln3_group(Gq, only=None):
                for i in (range(Gq * 4, Gq * 4 + 4) if only is None else [only]):
                    s_ = i % 2
                    xo = arena[:, i * 1024:(i + 1) * 1024]
                    rstd, nmr, B = ln_stats(xo, Bxn[i])
                    k.op("act", lambda: A.activation(out=outr[s_], in_=xo, func=AF.Identity, scale=rstd, bias=nmr), reads=[Bxn[i], B], writes=[Boutr[s_]])
                    k.op("dve", lambda: V.tensor_tensor(out=outr[s_], in0=outr[s_], in1=g3bc, op=ALU.mult), reads=[Boutr[s_], Bg3], writes=[Boutr[s_]])
                    k.op("dve", lambda: V.tensor_tensor(out=outr[s_], in0=outr[s_], in1=b3bc, op=ALU.add), reads=[Boutr[s_], Bb3], writes=[Boutr[s_]])
                    k.op("sp", lambda: SPQ.dma_start(out=y[b, i * 128:(i + 1) * 128, :], in_=outr[s_]), reads=[Boutr[s_]], dma=douts[s_])
            for e in range(16):
                W1, W3, W2, base = Wcur
                Wnext = None
                for Gi in range(NG):
                    tokG = slice(Gi * 512, (Gi + 1) * 512)
                    gi2 = gc_ % 2
                    if gc_ == 0:
                        cmb_prep(0, 0, 0)
                    gc_ += 1
                    H = hid[gi2]
                    for c in range(4):
                        Fr = fr[fc % 2]
                        FB = Fr["B"]
                        pa, Bpa = PA[fc % 2]
                        pb_, Bpb_ = PB[fc % 2]
                        fc += 1
                        cs = slice(c * 128, (c + 1) * 128)

                        def mmab(pp, W):
                            for kc in range(8):
                                ins = T.matmul(pp[:], lhsT=W[:, kc, cs], rhs=hT[:, kc, tokG], start=(kc == 0), stop=(kc == 7))
                            return ins
                        k.op("pe", lambda: mmab(pa, W1), reads=[BhT[Gi], Bw[base]], writes=[Bpa])
                        k.op("pe", lambda: mmab(pb_, W3), reads=[BhT[Gi], Bw[base + 1]], writes=[Bpb_])
                        if c == 0:
                            ne, nG = (e, Gi + 1) if Gi + 1 < NG else (e + 1, 0)
                            if ne < 16:
                                cmb_prep(ne, nG, gc_ % 2)
                        k.op("act", lambda: A.activation(out=Fr["sa"], in_=pa[:], func=AF.Silu), reads=[Bpa], writes=[FB["sa"]])
                        k.op("dve", lambda: V.tensor_tensor(out=Fr["t"], in0=Fr["sa"], in1=pb_[:], op=ALU.mult), reads=[FB["sa"], Bpb_], writes=[FB["t"]])
                        k.op("dve", lambda: V.tensor_tensor(out=H[:, c, :], in0=Fr["t"], in1=cmbs[gi2], op=ALU.mult),
                             reads=[FB["t"], Bcmbs[gi2]], writes=[Bhid[gi2]])
                    if pend is not None:
                        emit_y(pend[:5], last=(pend[5] == 15))
                    pend = (H, gi2, Gi, W2, base, e)
                    if Gi == 0 and e + 1 < 16:
                        Wnext = load_expert(e + 1)
                Wcur = Wnext
            emit_y(pend[:5], last=True)
            k.barrier()
        for kk, v in k.cnt.items():
            if v > 0:
                SPQ.wait_ge(k.sems[kk], v)
    return nc


def prep_inputs(inp, nb_per_core, ncores):
    f = lambda a: np.ascontiguousarray(np.asarray(a, dtype=np.float32))
    colv = np.zeros((128, 56), np.float32)
    for l, (g, bb) in enumerate(((inp["ln_in_g"], inp["ln_in_b"]), (inp["ln1_g"][0], inp["ln1_b"][0]), (inp["ln2_g"][0], inp["ln2_b"][0]))):
        colv[:, l * 16:l * 16 + 8] = np.asarray(g).reshape(8, 128).T
        colv[:, l * 16 + 8:l * 16 + 16] = np.asarray(bb).reshape(8, 128).T
    lbl = np.asarray(inp["hg_lb_logits"])
    colv[:, 48:52] = lbl[0].reshape(4, 128).T
    colv[:, 52:56] = lbl[1].reshape(4, 128).T
    rowv = np.stack([np.asarray(inp["ln_in_g"]), np.asarray(inp["ln1_g"][0]), np.asarray(inp["ln2_g"][0]), np.asarray(inp["ln3_g"][0]),
                     np.asarray(inp["ln3_b"][0]), np.asarray(inp["ln_in_b"]), np.asarray(inp["ln1_b"][0]), np.asarray(inp["ln2_b"][0])]).astype(np.float32)
    shared = {
        "w_in": f(inp["w_in"][0]), "w_branch_a": f(inp["w_branch_a"][0]), "w_branch_b": f(inp["w_branch_b"][0]),
        "w_mix_out": f(inp["w_mix_out"][0]), "xa_wq": f(inp["xa_wq"][0]), "xa_wk": f(inp["xa_wk"][0]), "xa_wv": f(inp["xa_wv"][0]),
        "xa_wo": f(inp["xa_wo"][0]), "moe_w1": f(inp["moe_w1"][0]), "moe_w3": f(inp["moe_w3"][0]), "moe_w2": f(inp["moe_w2"][0]),
        "cst": make_consts(), "colv": colv, "rowv": f(rowv), "ng": f(np.asarray(inp["hg_norm_g"]).reshape(1, 128)),
        "wr": f(np.concatenate([np.asarray(inp["router_wg"][0]), np.asarray(inp["router_we"][0])], axis=1)),
        "br": f(np.concatenate([np.asarray(inp["router_bg"][0]), np.asarray(inp["router_be"][0])]).reshape(1, 20)),
    }
    xs = f(inp["x"])
    ms = f(inp["mem"])
    maps = []
    for c in range(ncores):
        m = dict(shared)
        m["x"] = xs[c * nb_per_core:(c + 1) * nb_per_core]
        m["mem"] = ms[c * nb_per_core:(c + 1) * nb_per_core]
        maps.append(m)
    return maps


def kernel(**inputs):
    B, S, _ = inputs["x"].shape
    ncores = 8
    nb = B // ncores
    nc = build(S=S, NB=nb)
    maps = prep_inputs(inputs, nb, ncores)
    res = run_bass_kernel_spmd(nc, maps, core_ids=list(range(ncores)))
    return np.concatenate([r["y"] for r in res.results], axis=0).astype(np.float32)
```

```python
import os
import numpy as np
from contextlib import ExitStack
import concourse.bass as bass
import concourse.mybir as mybir
from concourse.bass_utils import run_bass_kernel_spmd

F32 = mybir.dt.float32
BF16 = mybir.dt.bfloat16
AF = mybir.ActivationFunctionType
ALU = mybir.AluOpType
AX = mybir.AxisListType

D = 1024
ALPHA = 2.0 ** 0.25
NCST = 2304


class Buf:
    __slots__ = ("w", "r")

    def __init__(self):
        self.w = None
        self.r = {}


class K:
    def __init__(self, nc, es):
        self.nc = nc
        self.es = es
        self.E = {"pe": nc.tensor, "act": nc.scalar, "dve": nc.vector, "pool": nc.gpsimd, "sp": nc.sync}
        self.sems = {}
        self.cnt = {}
        self.waited = {}
        for e in self.E:
            self.sems[e] = es.enter_context(nc.semaphore("s_" + e))
            self.cnt[e] = 0
        self.nd = 0
        self.INLINE = set(os.environ.get("KINLINE", "act,dve,pool,pe").split(","))
        self._pe_first = None
        pe = nc.tensor
        o_mm, o_tr = pe.matmul, pe.transpose

        def mm(*a, **kw):
            ins = o_mm(*a, **kw)
            if self._pe_first is None:
                self._pe_first = ins
            return ins

        def tr(*a, **kw):
            ins = o_tr(*a, **kw)
            if self._pe_first is None:
                self._pe_first = ins
            return ins
        pe.matmul = mm
        pe.transpose = tr

    def dsem(self, name):
        key = "d_%s_%d" % (name, self.nd)
        self.nd += 1
        self.sems[key] = self.es.enter_context(self.nc.semaphore(key))
        self.cnt[key] = 0
        return key

    def sb(self, name, shape, dt):
        return self.es.enter_context(self.nc.sbuf_tensor("sb_" + name, list(shape), dt))

    def ps(self, name, shape, dt=F32):
        return self.es.enter_context(self.nc.psum_tensor(name, list(shape), dt))

    def op(self, eng, fn, reads=(), writes=(), dma=None):
        def flat(xs):
            out = []
            for x in xs:
                if isinstance(x, (tuple, list)):
                    out.extend(flat(x))
                else:
                    out.append(x)
            return out
        reads = flat(reads)
        writes = flat(writes)
        deps = {}
        for b in reads:
            if b.w is not None:
                kk, v = b.w
                if deps.get(kk, 0) < v:
                    deps[kk] = v
        for b in writes:
            if b.w is not None:
                kk, v = b.w
                if deps.get(kk, 0) < v:
                    deps[kk] = v
            for kk, v in b.r.items():
                if deps.get(kk, 0) < v:
                    deps[kk] = v
        E = self.E[eng]
        need = [(kk, v) for kk, v in deps.items() if self.waited.get((eng, kk), 0) < v]
        inline = None
        if need and dma is None and eng in self.INLINE:
            inline = need.pop()
        for kk, v in need:
            E.wait_ge(self.sems[kk], v)
            self.waited[(eng, kk)] = v
        self._pe_first = None
        ins = fn()
        if inline is not None:
            tgt = self._pe_first if eng == "pe" else ins
            tgt._wait_ge(self.sems[inline[0]], inline[1])
            self.waited[(eng, inline[0])] = inline[1]
        if dma is None:
            key = eng
            self.cnt[key] += 1
            ins.then_inc(self.sems[key], 1)
        else:
            key = dma
            self.cnt[key] += 16
            ins.then_inc(self.sems[key], 16)
        v = self.cnt[key]
        for b in reads:
            if b.r.get(key, 0) < v:
                b.r[key] = v
        for b in writes:
            b.w = (key, v)
            b.r = {}
        return (key, v)

    def barrier(self):
        for eng, E in self.E.items():
            for kk, v in self.cnt.items():
                if v > 0 and self.waited.get((eng, kk), 0) < v:
                    E.wait_ge(self.sems[kk], v)
                    self.waited[(eng, kk)] = v


def make_consts():
    c = np.zeros((128, NCST), np.float32)
    j = np.arange(128)[:, None]
    s = np.arange(128)[None, :]
    c[:, 0:128] = np.eye(128)
    c[:, 128:256] = (j >= s)
    c[:, 256:384] = (j < s)
    c[:, 384:512] = (j < s)
    c[:, 512:640] = (j <= s) & ((j // 64) == (s // 64))
    t = np.arange(512)[None, :]
    c[:, 640:1152] = (t % 64 != 0)
    c[:, 1152:1664] = ((t // 64) % 2 == 0)
    c[:, 1664:2176] = ((t // 64) % 2 == 1)
    c[:, 2176:2304] = 1.0
    return c


def build(S=2048, NB=2, stop=None):
    NT = S // 128
    NG = S // 512
    nc = bass.Bass("TRN2", target_bir_lowering=False)

    def din(name, shape):
        return nc.dram_tensor(name, list(shape), F32, kind="ExternalInput").ap()

    x = din("x", [NB, S, D])
    mem = din("mem", [NB, 256, D])
    w_in = din("w_in", [D, 5632])
    wa_d = din("w_branch_a", [512, D])
    wb_d = din("w_branch_b", [512, D])
    wmix_d = din("w_mix_out", [D, D])
    wq_d = din("xa_wq", [D, D])
    wk_d = din("xa_wk", [D, D])
    wv_d = din("xa_wv", [D, D])
    wo_d = din("xa_wo", [D, D])
    w1_d = din("moe_w1", [16, D, 512])
    w3_d = din("moe_w3", [16, D, 512])
    w2_d = din("moe_w2", [16, 512, D])
    cst_d = din("cst", [128, NCST])
    colv_d = din("colv", [128, 56])
    rowv_d = din("rowv", [8, D])
    ng_d = din("ng", [1, 128])
    wr_d = din("wr", [D, 20])
    br_d = din("br", [1, 20])
    y = nc.dram_tensor("y", [NB, S, D], F32, kind="ExternalOutput").ap()
    dbg = None
    if stop is not None:
        dbg = nc.dram_tensor("dbg", [128, NT * 1024], F32, kind="ExternalOutput").ap()
        dbg2 = nc.dram_tensor("dbg2", [128, 8 * S], BF16, kind="ExternalOutput").ap()

    with ExitStack() as es:
        k = K(nc, es)
        T, V, A, G_, SPQ = nc.tensor, nc.vector, nc.scalar, nc.gpsimd, nc.sync

        SCR = 44 * 1024
        scr = k.sb("scr", [128, SCR // 4], F32)
        scrb = scr[:].bitcast(BF16)
        cst = scr[:, 0:NCST]
        Bcst0 = Buf()
        Bcst = Buf()
        cpf = k.sb("cpf", [128, 898], F32)
        idf = cpf[:, 0:128]
        dmask = cpf[:, 128:256]
        hmask = cpf[:, 256:384]
        resetm = cpf[:, 384:896]
        mlo = cpf[:, 896:897]
        mhi = cpf[:, 897:898]
        cbf = k.sb("cbf", [128, 1792], BF16)
        ident_bf = cbf[:, 1536:1664]
        neg_bf = cbf[:, 1664:1792]
        L_bf = cbf[:, 0:128]
        U_bf = cbf[:, 128:256]
        ones_bf = cbf[:, 256:384]
        zeros_bf = cbf[:, 384:512]
        evenm = cbf[:, 512:1024]
        oddm = cbf[:, 1024:1536]
        colv = k.sb("colv", [128, 56], F32)
        lbt = k.sb("lbt", [128, 16], F32)
        oml = lbt[:, 12:16]
        agbc = k.sb("agbc", [128, D], F32)
        Bagbc, Bg3 = Buf(), Buf()
        abbc = k.sb("abbc", [128, D], F32)
        cmbA = k.sb("cmbA", [128, NT, 16], F32)
        BcmbA = [Buf() for _ in range(NT)]
        ngbc4 = k.sb("ngbc4", [128, 512], F32)
        wr = scr[:, NCST:NCST + 160].rearrange("p (c n) -> p c n", c=8)
        brbc = k.sb("brbc", [128, 20], F32)
        wrb = k.sb("wrb", [128, 8, 20], BF16)
        hT = k.sb("hT", [128, 8, S], BF16)
        BhT = [(Buf(), Buf()) for _ in range(NG)]
        arena = k.sb("arena", [128, NT * 1024], F32)
        abf = arena[:].bitcast(BF16)
        oaT = abf[:, 0:4 * S].rearrange("p (c s) -> p c s", c=4)
        obT = abf[:, 4 * S:8 * S].rearrange("p (c s) -> p c s", c=4)
        mrg = abf[:, 8 * S:16 * S].rearrange("p (i c t) -> p i c t", i=NT, c=8)
        Bxn = [Buf() for _ in range(NT)]
        Boa = [Buf() for _ in range(NG)]
        Bob = [Buf() for _ in range(NG)]
        Bmrg = [Buf() for _ in range(NT)]
        NSLOT = 6
        wsl = [k.sb("wsl%d" % i, [128, 4096], BF16) for i in range(NSLOT)]
        Bw = [Buf() for _ in range(NSLOT)]
        dw = [k.dsem("w") for _ in range(NSLOT)]
        NST = 4
        stt = [k.sb("st%d" % i, [128, 32], F32) for i in range(NST)]
        Bst = [Buf() for _ in range(NST)]
        stc = [0]
        P = [k.ps("ps%d" % i, [128, 512], F32) for i in range(8)]
        BP = [Buf() for _ in range(8)]
        pc = [0]

        def nextP():
            i = pc[0] % 8
            pc[0] += 1
            return P[i], BP[i]

        dld = k.dsem("ld")
        dout = k.dsem("out")
        dxr = [k.dsem("xr") for _ in range(4)]
        douts = [k.dsem("o0"), k.dsem("o1")]

        def wload(slot, src, kc):
            view = wsl[slot][:].rearrange("p (c n) -> p c n", c=kc)
            k.op("pool", lambda: G_.dma_start(out=view, in_=src.rearrange("(c p) n -> p c n", p=128)),
                 writes=[Bw[slot]], dma=dw[slot])
            return view

        k.op("sp", lambda: SPQ.dma_start(out=cst, in_=cst_d), writes=[Bcst0], dma=k.dsem("cst"))
        k.op("act", lambda: A.copy(out=cpf[:, 0:128], in_=cst[:, 0:128]), reads=[Bcst0], writes=[Bcst])
        k.op("dve", lambda: V.tensor_copy(out=cpf[:, 128:384], in_=cst[:, 384:640]), reads=[Bcst0], writes=[Bcst])
        k.op("act", lambda: A.copy(out=cpf[:, 384:896], in_=cst[:, 640:1152]), reads=[Bcst0], writes=[Bcst])
        k.op("dve", lambda: V.tensor_copy(out=cpf[:, 896:897], in_=cst[:, 320:321]), reads=[Bcst0], writes=[Bcst])
        k.op("dve", lambda: V.tensor_copy(out=cpf[:, 897:898], in_=cst[:, 192:193]), reads=[Bcst0], writes=[Bcst])
        Bcol = Buf()
        k.op("sp", lambda: SPQ.dma_start(out=colv[:], in_=colv_d), writes=[Bcol], dma=k.dsem("colv"))
        Bwr = Buf()
        Bbrr = Buf()
        k.op("sp", lambda: SPQ.dma_start(out=wr, in_=wr_d.rearrange("(c p) n -> p c n", p=128)), writes=[Bwr], dma=k.dsem("wr"))
        k.op("sp", lambda: SPQ.dma_start(out=brbc[:], in_=br_d.partition_broadcast(128)), writes=[Bbrr], dma=k.dsem("brr"))
        k.op("act", lambda: A.copy(out=wrb[:], in_=wr), reads=[Bwr], writes=[Bwr])
        Bng = Buf()
        dng = k.dsem("ng")
        for hh in range(4):
            k.op("sp", lambda hh=hh: SPQ.dma_start(out=ngbc4[:, hh * 128:(hh + 1) * 128], in_=ng_d.partition_broadcast(128)),
                 writes=[Bng], dma=dng)
        dag, dab, dg3, db3 = k.dsem("ag"), k.dsem("ab"), k.dsem("g3"), k.dsem("b3")
        Babbc, Bb3 = Buf(), Buf()
        Bcbf = Buf()
        k.op("act", lambda: A.copy(out=cbf[:, 0:256], in_=cst[:, 128:384]), reads=[Bcst0], writes=[Bcbf])
        k.op("act", lambda: A.copy(out=cbf[:, 256:384], in_=cst[:, 2176:2304]), reads=[Bcst0], writes=[Bcbf])
        k.op("dve", lambda: V.memset(cbf[:, 384:512], 0.0), writes=[Bcbf])
        k.op("act", lambda: A.copy(out=cbf[:, 512:1536], in_=cst[:, 1152:2176]), reads=[Bcst0], writes=[Bcbf])
        k.op("act", lambda: A.copy(out=cbf[:, 1536:1664], in_=cst[:, 0:128]), reads=[Bcst0], writes=[Bcbf])
        k.op("dve", lambda: V.tensor_scalar(out=cbf[:, 1664:1792], in0=cst[:, 384:512], scalar1=-1.0, scalar2=30000.0, op0=ALU.add, op1=ALU.mult),
             reads=[Bcst0], writes=[Bcbf])
        k.op("act", lambda: A.activation(out=lbt[:, 0:8], in_=colv[:, 48:56], func=AF.Exp), reads=[Bcol], writes=[Bcol])
        k.op("dve", lambda: V.tensor_tensor(out=lbt[:, 8:12], in0=lbt[:, 0:4], in1=lbt[:, 4:8], op=ALU.add), reads=[Bcol], writes=[Bcol])
        k.op("dve", lambda: V.reciprocal(out=lbt[:, 8:12], in_=lbt[:, 8:12]), reads=[Bcol], writes=[Bcol])
        k.op("dve", lambda: V.tensor_tensor(out=lbt[:, 12:16], in0=lbt[:, 4:8], in1=lbt[:, 8:12], op=ALU.mult), reads=[Bcol], writes=[Bcol])

        def load_resid_consts(l):
            k.op("sp", lambda: SPQ.dma_start(out=agbc[:], in_=rowv_d[l:l + 1, :].partition_broadcast(128)), writes=[Bagbc], dma=dag)
            k.op("act", lambda: A.mul(out=agbc[:], in_=agbc[:], mul=ALPHA), reads=[Bagbc], writes=[Bagbc])
            k.op("sp", lambda: SPQ.dma_start(out=abbc[:], in_=rowv_d[5 + l:6 + l, :].partition_broadcast(128)), writes=[Babbc], dma=dab)
            k.op("act", lambda: A.mul(out=abbc[:], in_=abbc[:], mul=ALPHA), reads=[Babbc], writes=[Babbc])

        def ln_stats(src, Bsrc):
            i = stc[0] % NST
            stc[0] += 1
            st, B = stt[i], Bst[i]
            k.op("dve", lambda: V.bn_stats(out=st[:, 0:6], in_=src[:, 0:512]), reads=[Bsrc], writes=[B])
            k.op("dve", lambda: V.bn_stats(out=st[:, 6:12], in_=src[:, 512:1024]), reads=[Bsrc], writes=[B])
            k.op("dve", lambda: V.bn_aggr(out=st[:, 12:14], in_=st[:, 0:12]), reads=[B], writes=[B])
            k.op("act", lambda: A.activation(out=st[:, 14:15], in_=st[:, 13:14], func=AF.Ln, bias=epsc[:, 0:1], scale=1.0), reads=[B, Beps], writes=[B])
            k.op("act", lambda: A.activation(out=st[:, 15:16], in_=st[:, 14:15], func=AF.Exp, scale=-0.5), reads=[B], writes=[B])
            k.op("dve", lambda: V.scalar_tensor_tensor(out=st[:, 16:17], in0=st[:, 12:13], scalar=-1.0, in1=st[:, 15:16],
                                                      op0=ALU.mult, op1=ALU.mult), reads=[B], writes=[B])
            return st[:, 15:16], st[:, 16:17], B

        epsc = k.sb("epsc", [128, 4], F32)
        Beps = Buf()
        k.op("dve", lambda: V.memset(epsc[:, 0:1], 1e-5), writes=[Beps])
        k.op("dve", lambda: V.memset(epsc[:, 1:2], 1e-6), writes=[Beps])
        k.op("dve", lambda: V.memset(epsc[:, 2:3], 1.0), writes=[Beps])

        def transpose_evac(src, Bsrc, l, i, router=None):
            g = i // 4
            tok = slice(i * 128, (i + 1) * 128)
            for half in range(2):
                pb, Bpb = nextP()

                def tr():
                    for j in range(4):
                        c = half * 4 + j
                        ins = T.transpose(out=pb[:, j * 128:(j + 1) * 128], in_=src[:, c * 128:(c + 1) * 128], identity=idf)
                    return ins
                k.op("pe", tr, reads=[Bsrc, Bcst], writes=[Bpb])
                for j in range(4):
                    c = half * 4 + j
                    gc = colv[:, l * 16 + c:l * 16 + c + 1]
                    bc = colv[:, l * 16 + 8 + c:l * 16 + 8 + c + 1]
                    if half == 0:
                        k.op("act", lambda: A.activation(out=hT[:, c, tok], in_=pb[:, j * 128:(j + 1) * 128], func=AF.Identity,
                                                         scale=gc, bias=bc), reads=[Bpb, Bcol], writes=[BhT[g][0]])
                    else:
                        k.op("dve", lambda: V.tensor_scalar(out=hT[:, c, tok], in0=pb[:, j * 128:(j + 1) * 128], scalar1=gc, scalar2=bc,
                                                           op0=ALU.mult, op1=ALU.add), reads=[Bpb, Bcol], writes=[BhT[g][1]])
                    if router is not None:
                        hf, Bhf, lgp, Blg = router
                        k.op("dve" if j % 2 == 0 else "act",
                             (lambda: V.tensor_scalar(out=hf[:, c, :], in0=pb[:, j * 128:(j + 1) * 128], scalar1=gc, scalar2=bc,
                                                      op0=ALU.mult, op1=ALU.add)) if j % 2 == 0 else
                             (lambda: A.activation(out=hf[:, c, :], in_=pb[:, j * 128:(j + 1) * 128], func=AF.Identity, scale=gc, bias=bc)),
                             reads=[Bpb, Bcol], writes=[Bhf])

        def dump_and_stop(what):
            k.barrier()
            k.op("sp", lambda: SPQ.dma_start(out=dbg2, in_=hT[:].rearrange("p c s -> p (c s)")), dma=dout)
            k.op("sp", lambda: SPQ.dma_start(out=dbg, in_=arena[:]), dma=dout)

        xr = [scr[:, j * 1024:(j + 1) * 1024] for j in range(4)]
        Bxr = [Buf() for _ in range(4)]

        def load_ln_in(b, i, xnt, Bxnt, nring=2):
            s = i % nring
            k.op("sp", lambda: SPQ.dma_start(out=xr[s], in_=x[b, i * 128:(i + 1) * 128, :]), writes=[Bxr[s]], dma=dxr[s])
            rstd, nmr, B = ln_stats(xr[s], Bxr[s])
            k.op("act", lambda: A.activation(out=xnt, in_=xr[s], func=AF.Identity, scale=rstd, bias=nmr),
                 reads=[Bxr[s], B], writes=[Bxnt])

        k.barrier()
        if stop is not None:
            for i in range(NT):
                k.op("dve", lambda: V.memset(arena[:, i * 1024:(i + 1) * 1024], 0.0), writes=[Bxn[i]])
            k.barrier()
        for b in range(NB):
            Wq = wload(0, w_in[:, 0:512], 8)
            Wf = wload(1, w_in[:, 512:1024], 8)
            Wi = wload(2, w_in[:, 1024:1536], 8)
            Wg = wload(3, w_in[:, 1536:2048], 8)
            Wqs = wload(4, w_in[:, 2048:2560], 8)
            Wks = wload(5, w_in[:, 2560:3072], 8)
            xnA = [scr[:, (4 + j) * 1024:(5 + j) * 1024] for j in range(4)]
            BxnA = [Buf() for _ in range(4)]
            for i in range(NT):
                s = i % 4
                load_ln_in(b, i, xnA[s], BxnA[s], nring=4)
                if i >= 1:
                    transpose_evac(xnA[(i - 1) % 4], BxnA[(i - 1) % 4], 0, i - 1)
            transpose_evac(xnA[(NT - 1) % 4], BxnA[(NT - 1) % 4], 0, NT - 1)
            k.barrier()
            if stop == "A":
                dump_and_stop("hT")
                break

            o = 0

            def carve(nbytes, dt, shape=None):
                nonlocal o
                if dt == F32:
                    v = scr[:, o // 4:(o + nbytes) // 4]
                else:
                    v = scrb[:, o // 2:(o + nbytes) // 2]
                o += nbytes
                return v
            Vtok = carve(4096, BF16).rearrange("p (t n) -> p t n", t=4)
            gate2 = carve(8192, F32).rearrange("p (t n) -> p t n", t=4)
            BVtok, Bgate = Buf(), Buf()
            Sst = carve(4096, F32).rearrange("p (h t n) -> p h t n", h=4, t=2)
            BS = [[Buf(), Buf()] for _ in range(4)]
            FR = dict(e=carve(2048, F32), kk=carve(2048, F32), bc=carve(2048, F32), eb=carve(2048, F32))
            FB = {nm: Buf() for nm in ("e", "kk", "bc", "eb")}
            sgt, Bsg = FR["kk"], FB["kk"]
            BK = []
            for r in range(2):
                d = dict(
                    qe=carve(1024, BF16), qlo=carve(1024, BF16), qhi=carve(1024, BF16), keb=carve(1024, BF16),
                    ketok=carve(1024, BF16).rearrange("p (t n) -> p t n", t=4),
                    ketok2=carve(1024, BF16).rearrange("p (t n) -> p t n", t=4),
                    Sbf=carve(2048, BF16).rearrange("p (c n) -> p c n", c=8),
                    ebl=carve(32, F32),
                )
                d["B"] = {nm: Buf() for nm in ("qe", "qlo", "qhi", "keb", "ketok", "Sbf", "ebl")}
                BK.append(d)
            oring = []
            for r in range(3):
                d = dict(AT=carve(256, BF16), sq=carve(512, F32), of=carve(512, F32), ss=carve(16, F32))
                d["B"] = {nm: Buf() for nm in ("AT", "sq", "of", "ss")}
                oring.append(d)
            assert o <= SCR, o
            k.op("dve", lambda: V.memset(Sst.rearrange("p h t n -> p (h t n)"), 0.0), writes=[bb for pr in BS for bb in pr])
            occ = [0]

            def VG(Gi):
                for ti in range(4):
                    i = Gi * 4 + ti
                    tok = slice(i * 128, (i + 1) * 128)
                    pv, Bpv = nextP()

                    def mmv(pp, W):
                        for kc in range(8):
                            ins = T.matmul(pp[:], lhsT=hT[:, kc, tok], rhs=W[:, kc, :], start=(kc == 0), stop=(kc == 7))
                        return ins
                    k.op("pe", lambda: mmv(pv, Wi), reads=[BhT[Gi], Bw[2]], writes=[Bpv])
                    k.op("act", lambda: A.copy(out=Vtok[:, ti, :], in_=pv[:]), reads=[Bpv], writes=[BVtok])
                    pg, Bpg = nextP()
                    k.op("pe", lambda: mmv(pg, Wg), reads=[BhT[Gi], Bw[3]], writes=[Bpg])
                    k.op("act", lambda: A.activation(out=sgt, in_=pg[:], func=AF.Silu), reads=[Bpg], writes=[Bsg])
                    k.op("dve", lambda: V.tensor_tensor(out=gate2[:, ti, :], in0=sgt, in1=ngbc4[:], op=ALU.mult),
                         reads=[Bsg, Bng], writes=[Bgate])

            def front(Gi, h, Kb):
                KB = Kb["B"]
                tokG = slice(Gi * 512, (Gi + 1) * 512)
                hs = slice(h * 128, (h + 1) * 128)
                pq, Bpq = nextP()

                def mmf(pp, W):
                    for kc in range(8):
                        ins = T.matmul(pp[:], lhsT=W[:, kc, hs], rhs=hT[:, kc, tokG], start=(kc == 0), stop=(kc == 7))
                    return ins
                k.op("pe", lambda: mmf(pq, Wq), reads=[BhT[Gi], Bw[0]], writes=[Bpq])
                yield
                pf, Bpf = nextP()
                k.op("pe", lambda: mmf(pf, Wf), reads=[BhT[Gi], Bw[1]], writes=[Bpf])
                yield
                e, kk, bc, eb = FR["e"], FR["kk"], FR["bc"], FR["eb"]
                k.op("act", lambda: A.activation(out=e, in_=pf[:], func=AF.Exp), reads=[Bpf], writes=[FB["e"]])
                yield
                k.op("act", lambda: A.activation(out=e, in_=e, func=AF.Ln, bias=1.0, scale=1.0), reads=[FB["e"], Beps], writes=[FB["e"]])
                yield
                k.op("act", lambda: A.activation(out=e, in_=e, func=AF.Exp, scale=-1.0), reads=[FB["e"]], writes=[FB["e"]])
                yield
                k.op("dve", lambda: V.tensor_scalar(out=kk, in0=e, scalar1=oml[:, h:h + 1], scalar2=None, op0=ALU.mult),
                     reads=[FB["e"], Bcol], writes=[FB["kk"]])
                yield
                k.op("act", lambda: A.activation(out=e, in_=kk, func=AF.Ln, scale=-1.0, bias=1.0),
                     reads=[FB["kk"], Beps], writes=[FB["e"]])
                yield
                k.op("dve", lambda: V.tensor_tensor_scan(out=bc, data0=resetm, data1=e, initial=0.0,
                                                        op0=ALU.mult, op1=ALU.add), reads=[FB["e"], Bcst], writes=[FB["bc"]])
                yield
                k.op("act", lambda: A.activation(out=eb, in_=bc, func=AF.Exp), reads=[FB["bc"]], writes=[FB["eb"]])
                yield
                k.op("act", lambda: A.activation(out=e, in_=bc, func=AF.Exp, scale=-1.0), reads=[FB["bc"]], writes=[FB["e"]])
                yield
                k.op("dve", lambda: V.tensor_tensor(out=Kb["qe"], in0=pq[:], in1=eb, op=ALU.mult), reads=[Bpq, FB["eb"]], writes=[KB["qe"]])
                yield
                k.op("dve", lambda: V.tensor_copy(out=Kb["ebl"], in_=eb.rearrange("p (c t) -> p c t", t=64)[:, :, 63]),
                     reads=[FB["eb"]], writes=[KB["ebl"]])
                yield
                k.op("dve", lambda: V.tensor_tensor(out=e, in0=kk, in1=e, op=ALU.mult), reads=[FB["kk"], FB["e"]], writes=[FB["e"]])
                yield
                k.op("dve", lambda: V.tensor_tensor(out=Kb["qlo"], in0=Kb["qe"], in1=evenm, op=ALU.mult), reads=[KB["qe"], Bcbf], writes=[KB["qlo"]])
                yield
                k.op("dve", lambda: V.tensor_tensor(out=Kb["qhi"], in0=Kb["qe"], in1=oddm, op=ALU.mult), reads=[KB["qe"], Bcbf], writes=[KB["qhi"]])
                yield
                k.op("act", lambda: A.copy(out=Kb["keb"], in_=e), reads=[FB["e"]], writes=[KB["keb"]])
                yield
                pt, Bpt = nextP()

                def trk():
                    for j in range(4):
                        ins = T.transpose(out=pt[:, j * 128:(j + 1) * 128], in_=e[:, j * 128:(j + 1) * 128], identity=idf)
                    return ins
                k.op("pe", trk, reads=[FB["e"], Bcst], writes=[Bpt])
                yield
                k.op("act", lambda: A.activation(out=Kb["ketok"].rearrange("p t n -> p (t n)"), in_=pt[:], func=AF.Identity, scale=mlo),
                     reads=[Bpt, Bcst], writes=[KB["ketok"]])
                yield
                k.op("dve", lambda: V.tensor_scalar(out=Kb["ketok2"].rearrange("p t n -> p (t n)"), in0=pt[:], scalar1=mhi, scalar2=None, op0=ALU.mult),
                     reads=[Bpt, Bcst], writes=[KB["ketok"]])
                yield

            def back(Gi, h, Kb):
                KB = Kb["B"]
                hs = slice(h * 128, (h + 1) * 128)
                pm = [nextP(), nextP()]
                for hf_ in range(2):
                    def mmm():
                        for cc in range(4):
                            c = hf_ * 4 + cc
                            j = c // 2
                            kt = Kb["ketok"] if c % 2 == 0 else Kb["ketok2"]
                            ins = T.matmul(pm[hf_][0][:, cc * 128:(cc + 1) * 128], lhsT=kt[:, j, :], rhs=Vtok[:, j, hs],
                                           start=True, stop=True)
                        return ins
                    k.op("pe", mmm, reads=[KB["ketok"], BVtok], writes=[pm[hf_][1]])
                    yield
                for c in range(8):
                    pmc = pm[c // 4][0][:, (c % 4) * 128:(c % 4 + 1) * 128]
                    k.op("act", lambda: A.activation(out=pmc, in_=pmc, func=AF.Identity, scale=Kb["ebl"][:, c:c + 1]),
                         reads=[pm[c // 4][1], KB["ebl"]], writes=[pm[c // 4][1]])
                    yield
                k.op("act", lambda: A.copy(out=Kb["Sbf"][:, 0, :], in_=Sst[:, h, 0, :]), reads=[BS[h][0]], writes=[KB["Sbf"]])
                yield
                for c in range(8):
                    pmc = pm[c // 4][0][:, (c % 4) * 128:(c % 4 + 1) * 128]
                    src, dst = Sst[:, h, c % 2, :], Sst[:, h, (c + 1) % 2, :]
                    k.op("dve", lambda: V.scalar_tensor_tensor(out=dst, in0=src, scalar=Kb["ebl"][:, c:c + 1], in1=pmc, op0=ALU.mult, op1=ALU.add),
                         reads=[BS[h][c % 2], pm[c // 4][1], KB["ebl"]], writes=[BS[h][(c + 1) % 2]])
                    yield
                    if c < 7:
                        k.op("act", lambda: A.copy(out=Kb["Sbf"][:, c + 1, :], in_=dst), reads=[BS[h][(c + 1) % 2]], writes=[KB["Sbf"]])
                        yield
                st = {}

                def X(j):
                    O = oring[occ[0] % 3]
                    occ[0] += 1
                    OB = O["B"]
                    js = slice(j * 128, (j + 1) * 128)
                    pa, Bpa = nextP()
                    k.op("pe", lambda: T.matmul(pa[:, 0:128], lhsT=Kb["keb"][:, js], rhs=Kb["qe"][:, js], start=True, stop=True),
                         reads=[KB["keb"], KB["qe"]], writes=[Bpa])
                    yield
                    k.op("dve", lambda: V.tensor_tensor(out=O["AT"], in0=pa[:, 0:128], in1=hmask, op=ALU.mult), reads=[Bpa, Bcst], writes=[OB["AT"]])
                    yield
                    po, Bpo = nextP()

                    def mmo():
                        T.matmul(po[:, 0:128], lhsT=O["AT"], rhs=Vtok[:, j, hs], start=True, stop=False)
                        T.matmul(po[:, 0:128], lhsT=Kb["qlo"][:, js], rhs=Kb["Sbf"][:, 2 * j, :], start=False, stop=False)
                        return T.matmul(po[:, 0:128], lhsT=Kb["qhi"][:, js], rhs=Kb["Sbf"][:, 2 * j + 1, :], start=False, stop=True)
                    k.op("pe", mmo, reads=[OB["AT"], BVtok, KB["qlo"], KB["qhi"], KB["Sbf"]], writes=[Bpo])
                    yield
                    st[j] = (O, po, Bpo)

                def Y(j):
                    O, po, Bpo = st[j]
                    OB = O["B"]
                    hs_ = hs
                    k.op("act", lambda: A.activation(out=O["sq"], in_=po[:, 0:128], func=AF.Square), reads=[Bpo], writes=[OB["sq"]])
                    yield
                    k.op("dve", lambda: V.reduce_sum(out=O["ss"][:, 0:1], in_=O["sq"], axis=AX.X), reads=[OB["sq"]], writes=[OB["ss"]])
                    yield
                    k.op("act", lambda: A.activation(out=O["ss"][:, 1:2], in_=O["ss"][:, 0:1], func=AF.Ln, scale=1.0 / 128.0, bias=epsc[:, 1:2]),
                         reads=[OB["ss"], Beps], writes=[OB["ss"]])
                    yield
                    k.op("act", lambda: A.activation(out=O["ss"][:, 2:3], in_=O["ss"][:, 1:2], func=AF.Exp, scale=-0.5), reads=[OB["ss"]], writes=[OB["ss"]])
                    yield
                    k.op("dve", lambda: V.scalar_tensor_tensor(out=O["of"], in0=po[:, 0:128], scalar=O["ss"][:, 2:3], in1=gate2[:, j, hs_],
                                                              op0=ALU.mult, op1=ALU.mult), reads=[Bpo, OB["ss"], Bgate], writes=[OB["of"]])
                    yield

                def Z(j):
                    O, po, Bpo = st[j]
                    OB = O["B"]
                    p2, Bp2 = nextP()
                    k.op("pe", lambda: T.transpose(out=p2[:, 0:128], in_=O["of"], identity=idf), reads=[OB["of"], Bcst], writes=[Bp2])
                    yield
                    k.op("act", lambda: A.copy(out=oaT[:, h, Gi * 512 + j * 128:Gi * 512 + (j + 1) * 128], in_=p2[:, 0:128]),
                         reads=[Bp2], writes=[Boa[Gi]])
                    yield
                for fn_, j_ in ((X, 0), (X, 1), (Y, 0), (X, 2), (Y, 1), (Z, 0), (X, 3), (Y, 2), (Z, 1), (Y, 3), (Z, 2), (Z, 3)):
                    yield from fn_(j_)

            def run_il(*gens):
                gens = list(gens)
                while gens:
                    for g_ in list(gens):
                        try:
                            next(g_)
                        except StopIteration:
                            gens.remove(g_)
            def run_w(main, side, ratio):
                n = 0
                alive = True
                for _ in main:
                    n += 1
                    if alive and n % ratio == 0:
                        try:
                            next(side)
                        except StopIteration:
                            alive = False
                if alive:
                    for _ in side:
                        pass
            RW = int(os.environ.get("KRW", "3"))
            itb = 0
            VG(0)
            run_il(front(0, 0, BK[0]))
            for Gi in range(NG):
                for h in range(4):
                    cur = BK[itb % 2]
                    if h + 1 < 4:
                        run_w(back(Gi, h, cur), front(Gi, h + 1, BK[(itb + 1) % 2]), RW)
                    elif Gi + 1 < NG:
                        run_w(back(Gi, h, cur), front(Gi + 1, 0, BK[(itb + 1) % 2]), RW)
                        VG(Gi + 1)
                    else:
                        run_il(back(Gi, h, cur))
                    itb += 1
            k.barrier()
            if stop == "B":
                dump_and_stop("arena")
                break

            Wvs = wload(0, w_in[:, 3072:3584], 8)
            Wa = wload(1, wa_d, 4)
            Wb = wload(2, wb_d, 4)
            Wga0 = wload(3, w_in[:, 3584:4096], 8)
            o = 0
            qTp = carve(2 * S, BF16)
            kTp = [carve(2 * S, BF16), carve(2 * S, BF16)]
            Vpad = carve(NT * 512, BF16).rearrange("p (i u n) -> p i u n", i=NT, u=2)
            BqT, BkT, BVp = Buf(), Buf(), (Buf(), Buf())
            cr = []
            for u in range(2):
                for par in range(2):
                    d = dict(e=carve(2048, F32), sp=carve(1024, BF16), et=carve(2048, F32), WT=carve(1024, BF16))
                    d["B"] = {nm: Buf() for nm in ("e", "sp", "et", "WT")}
                    cr.append(d)
            assert o <= SCR, o
            k.op("pool", lambda: G_.memset(Vpad.rearrange("p i u n -> p (i u n)"), 0.0), writes=[BVp])
            for hp in range(4):
                hps = slice(hp * 128, (hp + 1) * 128)
                for Gi in range(NG):
                    tokG = slice(Gi * 512, (Gi + 1) * 512)
                    for (W, wi, dst, Bd, eng) in ((Wqs, 4, qTp, BqT, "act"), (Wks, 5, kTp, BkT, "dve")):
                        pp, Bpp = nextP()

                        def mmp(pp=pp, W=W):
                            for kc in range(8):
                                ins = T.matmul(pp[:], lhsT=W[:, kc, hps], rhs=hT[:, kc, tokG], start=(kc == 0), stop=(kc == 7))
                            return ins
                        k.op("pe", mmp, reads=[BhT[Gi], Bw[wi]], writes=[Bpp])
                        if eng == "act":
                            k.op("act", lambda: A.copy(out=dst[:, tokG], in_=pp[:]), reads=[Bpp], writes=[Bd])
                        else:
                            k.op("dve", lambda: V.tensor_scalar(out=dst[0][:, tokG], in0=pp[:], scalar1=mlo, scalar2=None, op0=ALU.mult),
                                 reads=[Bpp, Bcst], writes=[Bd])
                            k.op("dve", lambda: V.tensor_scalar(out=dst[1][:, tokG], in0=pp[:], scalar1=mhi, scalar2=None, op0=ALU.mult),
                                 reads=[Bpp, Bcst], writes=[Bd])
                for i in range(NT):
                    tok = slice(i * 128, (i + 1) * 128)
                    pp, Bpp = nextP()

                    def mmv2():
                        for kc in range(8):
                            ins = T.matmul(pp[:, 0:128], lhsT=hT[:, kc, tok], rhs=Wvs[:, kc, hps], start=(kc == 0), stop=(kc == 7))
                        return ins
                    k.op("pe", mmv2, reads=[BhT[i // 4], Bw[0]], writes=[Bpp])
                    if i % 2 == 0:
                        k.op("act", lambda: A.copy(out=Vpad[:, i, 0, 0:64], in_=pp[:, 0:64]), reads=[Bpp], writes=[BVp[0]])
                        k.op("act", lambda: A.copy(out=Vpad[:, i, 1, 64:128], in_=pp[:, 64:128]), reads=[Bpp], writes=[BVp[0]])
                    else:
                        k.op("dve", lambda: V.tensor_copy(out=Vpad[:, i, 0, 0:64], in_=pp[:, 0:64]), reads=[Bpp], writes=[BVp[1]])
                        k.op("dve", lambda: V.tensor_copy(out=Vpad[:, i, 1, 64:128], in_=pp[:, 64:128]), reads=[Bpp], writes=[BVp[1]])
                Z = [(P[0], BP[0]), (P[1], BP[1]), (P[2], BP[2]), (P[3], BP[3])]
                Tb = [(P[4], BP[4]), (P[5], BP[5])]
                oTb = (P[6], BP[6])
                for qc in range(NG):
                    qs0 = qc * 512
                    for u in range(2):
                        k.op("pe", lambda: T.matmul(Tb[u][0][:], lhsT=zeros_bf, rhs=qTp[:, qs0:qs0 + 512], start=True, stop=True),
                             reads=[Bcbf, BqT], writes=[Tb[u][1]])
                    k.op("pe", lambda: T.matmul(oTb[0][:], lhsT=zeros_bf, rhs=qTp[:, qs0:qs0 + 512], start=True, stop=True),
                         reads=[Bcbf, BqT], writes=[oTb[1]])
                    steps = list(range(qc * 4 + 3, -1, -1))

                    def zmm(si):
                        kb = steps[si]
                        c0 = max(0, kb - qc * 4) * 128
                        w = 512 - c0
                        for u in range(2):
                            zt, Bz = Z[(si % 2) * 2 + u]
                            def zz():
                                ins = T.matmul(zt[:, 0:w], lhsT=kTp[u][:, kb * 128:(kb + 1) * 128], rhs=qTp[:, qs0 + c0:qs0 + 512],
                                               start=True, stop=True)
                                if kb >= qc * 4:
                                    ins = T.matmul(zt[:, 0:128], lhsT=ident_bf, rhs=neg_bf, start=False, stop=True, skip_group_check=True)
                                return ins
                            k.op("pe", zz, reads=[BkT, BqT, Bcbf], writes=[Bz])
                    def geom(si):
                        kb = steps[si]
                        c0 = max(0, kb - qc * 4) * 128
                        return kb, c0, 512 - c0, kb >= qc * 4

                    def stA(si):
                        kb, c0, w, diag = geom(si)
                        for u in range(2):
                            zt, Bz = Z[(si % 2) * 2 + u]
                            C = cr[u * 2 + si % 2]
                            CB = C["B"]
                            k.op("act", lambda: A.activation(out=C["e"][:, 0:w], in_=zt[:, 0:w], func=AF.Exp, scale=0.125), reads=[Bz], writes=[CB["e"]])
                            k.op("act", lambda: A.activation(out=C["sp"][:, 0:w], in_=C["e"][:, 0:w], func=AF.Ln, bias=1.0, scale=1.0),
                                 reads=[CB["e"], Beps], writes=[CB["sp"]])

                    def stL(si):
                        kb, c0, w, diag = geom(si)
                        for u in range(2):
                            C = cr[u * 2 + si % 2]
                            CB = C["B"]
                            k.op("pe", lambda: T.matmul(Tb[u][0][:, c0:512], lhsT=L_bf, rhs=C["sp"][:, 0:w], start=False, stop=True, skip_group_check=True),
                                 reads=[Bcbf, CB["sp"]], writes=[Tb[u][1]])

                    def stTail(si):
                        kb, c0, w, diag = geom(si)
                        for u in range(2):
                            C = cr[u * 2 + si % 2]
                            CB = C["B"]
                            k.op("act", lambda: A.activation(out=C["et"][:, 0:w], in_=Tb[u][0][:, c0:512], func=AF.Exp, scale=-1.0),
                                 reads=[Tb[u][1]], writes=[CB["et"]])
                            k.op("dve", lambda: V.tensor_tensor(out=C["WT"][:, 0:w], in0=C["e"][:, 0:w], in1=C["et"][:, 0:w], op=ALU.mult),
                                 reads=[CB["e"], CB["et"]], writes=[CB["WT"]])
                        for u in range(2):
                            C = cr[u * 2 + si % 2]
                            CB = C["B"]
                            k.op("pe", lambda: T.matmul(Tb[u][0][:, c0:512], lhsT=U_bf, rhs=C["sp"][:, 0:w], start=False, stop=True, skip_group_check=True),
                                 reads=[Bcbf, CB["sp"]], writes=[Tb[u][1]])
                            k.op("pe", lambda: T.matmul(oTb[0][:, c0:512], lhsT=Vpad[:, kb, u, :], rhs=C["WT"][:, 0:w], start=False, stop=True, skip_group_check=True),
                                 reads=[BVp, CB["WT"]], writes=[oTb[1]])
                    zmm(0)
                    for si in range(len(steps)):
                        stA(si)
                        if si + 1 < len(steps):
                            zmm(si + 1)
                        stL(si)
                        stTail(si)
                    k.op("act", lambda: A.copy(out=obT[:, hp, qs0:qs0 + 512], in_=oTb[0][:]), reads=[oTb[1]], writes=[Bob[qc]])
            k.barrier()
            if stop == "C":
                dump_and_stop("arena")
                break

            Wgb0 = wload(4, w_in[:, 4608:5120], 8)
            Wga1 = wload(5, w_in[:, 4096:4608], 8)
            Wgb1 = wload(0, w_in[:, 5120:5632], 8)
            o = 0
            dr = []
            for r in range(2):
                d = dict(sga=carve(2048, F32), sgb=carve(2048, F32), t1=carve(2048, F32), t2=carve(2048, F32))
                d["B"] = {nm: Buf() for nm in ("sga", "sgb", "t1", "t2")}
                dr.append(d)
            assert o <= SCR
            dc_ = 0
            Wmx = [None, None]
            for q in range(2):
                Wga, iga = (Wga0, 3) if q == 0 else (Wga1, 5)
                Wgb, igb = (Wgb0, 4) if q == 0 else (Wgb1, 0)
                for Gi in range(NG):
                    tokG = slice(Gi * 512, (Gi + 1) * 512)
                    for dc in range(4):
                        Rr = dr[dc_ % 2]
                        RB = Rr["B"]
                        dc_ += 1
                        cs = slice(dc * 128, (dc + 1) * 128)
                        cs2 = slice(q * 512 + dc * 128, q * 512 + (dc + 1) * 128)
                        pga, Bpga = nextP()
                        pgb, Bpgb = nextP()
                        pya, Bpya = nextP()
                        pyb, Bpyb = nextP()

                        def mm8(pp, W, csl):
                            for kc in range(8):
                                ins = T.matmul(pp[:], lhsT=W[:, kc, csl], rhs=hT[:, kc, tokG], start=(kc == 0), stop=(kc == 7))
                            return ins

                        def mm4(pp, W, src):
                            for kc in range(4):
                                ins = T.matmul(pp[:], lhsT=W[:, kc, cs2], rhs=src[:, kc, tokG], start=(kc == 0), stop=(kc == 3))
                            return ins
                        k.op("pe", lambda: mm8(pga, Wga, cs), reads=[BhT[Gi], Bw[iga]], writes=[Bpga])
                        k.op("pe", lambda: mm8(pgb, Wgb, cs), reads=[BhT[Gi], Bw[igb]], writes=[Bpgb])
                        k.op("pe", lambda: mm4(pya, Wa, oaT), reads=[Boa[Gi], Bw[1]], writes=[Bpya])
                        k.op("pe", lambda: mm4(pyb, Wb, obT), reads=[Bob[Gi], Bw[2]], writes=[Bpyb])
                        k.op("act", lambda: A.activation(out=Rr["sga"], in_=pga[:], func=AF.Sigmoid), reads=[Bpga], writes=[RB["sga"]])
                        k.op("act", lambda: A.activation(out=Rr["sgb"], in_=pgb[:], func=AF.Sigmoid), reads=[Bpgb], writes=[RB["sgb"]])
                        k.op("dve", lambda: V.tensor_tensor(out=Rr["t1"], in0=Rr["sga"], in1=pya[:], op=ALU.mult), reads=[RB["sga"], Bpya], writes=[RB["t1"]])
                        k.op("dve", lambda: V.tensor_tensor(out=Rr["t2"], in0=Rr["sgb"], in1=pyb[:], op=ALU.mult), reads=[RB["sgb"], Bpyb], writes=[RB["t2"]])
                        k.op("dve", lambda: V.tensor_tensor(out=mrg[:, Gi * 4:(Gi + 1) * 4, q * 4 + dc, :],
                                                              in0=Rr["t1"].rearrange("p (t n) -> p t n", t=4),
                                                              in1=Rr["t2"].rearrange("p (t n) -> p t n", t=4), op=ALU.add),
                             reads=[RB["t1"], RB["t2"]], writes=[Bmrg[Gi * 4 + tt] for tt in range(4)])
                if q == 0:
                    Wmx[0] = wload(3, wmix_d[:, 0:512], 8)
                    Wmx[1] = wload(4, wmix_d[:, 512:1024], 8)
            k.barrier()
            if stop == "D1":
                dump_and_stop("arena")
                break

            def resid_a(i, xprev, Bxprev, lhs_fn, lhs_reads, Wn, Wn_slots, nk, ut, But):
                banks = [nextP(), nextP()]
                for n in range(2):
                    pp, Bpp = banks[n]

                    def mmr():
                        for kc in range(nk):
                            ins = T.matmul(pp[:], lhsT=lhs_fn(kc), rhs=Wn[n][:, kc, :], start=(kc == 0), stop=(kc == nk - 1))
                        return ins
                    k.op("pe", mmr, reads=list(lhs_reads) + [Bw[Wn_slots[n]]], writes=[Bpp])
                k.op("pool", lambda: G_.tensor_tensor(out=ut, in0=xprev, in1=agbc[:], op=ALU.mult), reads=[Bxprev, Bagbc], writes=[But])
                k.op("pool", lambda: G_.tensor_tensor(out=ut, in0=ut, in1=abbc[:], op=ALU.add), reads=[But, Babbc], writes=[But])
                return banks

            def resid_b(i, banks, ut, But):
                for n in range(2):
                    pp, Bpp = banks[n]
                    ns = slice(n * 512, (n + 1) * 512)
                    k.op("dve", lambda: V.tensor_tensor(out=ut[:, ns], in0=ut[:, ns], in1=pp[:], op=ALU.add), reads=[But, Bpp], writes=[But])
                rstd, nmr, B = ln_stats(ut, But)
                xo = arena[:, i * 1024:(i + 1) * 1024]
                k.op("act", lambda: A.activation(out=xo, in_=ut, func=AF.Identity, scale=rstd, bias=nmr), reads=[But, B], writes=[Bxn[i]])

            load_resid_consts(0)
            Wk_ = [wload(5, wk_d[:, 0:512], 8), wload(0, wk_d[:, 512:1024], 8)]
            Wv_ = [wload(1, wv_d[:, 0:512], 8), wload(2, wv_d[:, 512:1024], 8)]
            utl = [scr[:, (8 + j) * 1024:(9 + j) * 1024] for j in range(3)]
            Butl = [Buf(), Buf(), Buf()]
            def d2_a(i):
                s = i % 3
                return resid_a(i, xnA[s], BxnA[s], lambda kc: mrg[:, i, kc, :], [Bmrg[i]], Wmx, (3, 4), 8, utl[s], Butl[s])
            load_ln_in(b, 0, xnA[0], BxnA[0], nring=3)
            if NT > 1:
                load_ln_in(b, 1, xnA[1], BxnA[1], nring=3)
            bk = {0: d2_a(0)}
            for i in range(NT):
                if i + 2 < NT:
                    load_ln_in(b, i + 2, xnA[(i + 2) % 3], BxnA[(i + 2) % 3], nring=3)
                if i + 1 < NT:
                    bk[i + 1] = d2_a(i + 1)
                resid_b(i, bk.pop(i), utl[i % 3], Butl[i % 3])
                if i >= 1:
                    transpose_evac(arena[:, (i - 1) * 1024:i * 1024], Bxn[i - 1], 1, i - 1)
            transpose_evac(arena[:, (NT - 1) * 1024:NT * 1024], Bxn[NT - 1], 1, NT - 1)
            k.barrier()
            if stop == "D2":
                dump_and_stop("arena")
                break

            load_resid_consts(1)
            Wq_ = [wload(3, wq_d[:, 0:512], 8), wload(4, wq_d[:, 512:1024], 8)]
            o = 8192
            hf = carve(8 * 128 * 4, F32).rearrange("p (c t) -> p c t", c=8)
            memT = hf.rearrange("p c t -> p (c t)").bitcast(BF16)[:, 0:2048].rearrange("p (c m) -> p c m", c=8)
            kTm = carve(8 * 256 * 2, BF16).rearrange("p (c m) -> p c m", c=8)
            vm = carve(2 * 1024 * 2, BF16).rearrange("p (t n) -> p t n", t=2)
            qTg = carve(8 * 512 * 2, BF16).rearrange("p (c t) -> p c t", c=8)
            oTg = carve(8 * 512 * 2, BF16).rearrange("p (c t) -> p c t", c=8)
            pTm = [carve(1024, BF16) for _ in range(4)]
            rinv = carve(2048, F32)
            rtb = [carve(448, F32) for _ in range(4)]
            Brtb = [Buf() for _ in range(4)]
            Blgb = [Buf() for _ in range(4)]
            assert o <= SCR, o
            utl = [scr[:, 0:1024], scr[:, 1024:2048]]
            BmemT, BkTm, Bvm, BqTg, BoTg, Brinv, Bhf = [Buf() for _ in range(7)]
            rinvs = [(rinv, Brinv), (hf.rearrange("p c t -> p (c t)")[:, 0:512], Buf())]
            BpTm = [Buf() for _ in range(4)]
            for mt in range(2):
                k.op("sp", lambda: SPQ.dma_start(out=xr[mt], in_=mem[b, mt * 128:(mt + 1) * 128, :]), writes=[Bxr[mt]], dma=dxr[mt])
                for half in range(2):
                    pb, Bpb = nextP()

                    def trm():
                        for j in range(4):
                            c = half * 4 + j
                            ins = T.transpose(out=pb[:, j * 128:(j + 1) * 128], in_=xr[mt][:, c * 128:(c + 1) * 128], identity=idf)
                        return ins
                    k.op("pe", trm, reads=[Bxr[mt], Bcst], writes=[Bpb])
                    k.op("act", lambda: A.copy(out=memT[:, half * 4:(half + 1) * 4, mt * 128:(mt + 1) * 128],
                                               in_=pb[:].rearrange("p (j t) -> p j t", j=4)), reads=[Bpb], writes=[BmemT])
            for c in range(8):
                pp, Bpp = nextP()
                Wkk = Wk_[c // 4]
                cs = slice((c % 4) * 128, (c % 4 + 1) * 128)

                def mmk():
                    for kc in range(8):
                        ins = T.matmul(pp[:, 0:256], lhsT=Wkk[:, kc, cs], rhs=memT[:, kc, :], start=(kc == 0), stop=(kc == 7))
                    return ins
                k.op("pe", mmk, reads=[BmemT, Bw[5 if c < 4 else 0]], writes=[Bpp])
                k.op("act" if c % 2 == 0 else "dve",
                     (lambda: A.copy(out=kTm[:, c, :], in_=pp[:, 0:256])) if c % 2 == 0 else (lambda: V.tensor_copy(out=kTm[:, c, :], in_=pp[:, 0:256])),
                     reads=[Bpp], writes=[BkTm])
            for mt in range(2):
                for n in range(2):
                    pp, Bpp = nextP()

                    def mmvv():
                        for kc in range(8):
                            ins = T.matmul(pp[:], lhsT=memT[:, kc, mt * 128:(mt + 1) * 128], rhs=Wv_[n][:, kc, :], start=(kc == 0), stop=(kc == 7))
                        return ins
                    k.op("pe", mmvv, reads=[BmemT, Bw[1 + n]], writes=[Bpp])
                    k.op("act", lambda: A.copy(out=vm[:, mt, n * 512:(n + 1) * 512], in_=pp[:]), reads=[Bpp], writes=[Bvm])
            k.barrier()
            Wo_ = [wload(5, wo_d[:, 0:512], 8), wload(0, wo_d[:, 512:1024], 8)]
            def router_tile(i, Gi, rt, Brt, lgs, Blgs):
                pl, Bpl = nextP()

                def mmrt():
                    for kc in range(8):
                        ins = T.matmul(pl[:, 0:20], lhsT=hT[:, kc, i * 128:(i + 1) * 128], rhs=wrb[:, kc, :], start=(kc == 0), stop=(kc == 7))
                    return ins
                k.op("pe", mmrt, reads=[BhT[Gi], Bwr], writes=[Bpl])
                yield
                k.op("dve", lambda: V.tensor_tensor(out=lgs, in0=pl[:, 0:20], in1=brbc[:], op=ALU.add), reads=[Bpl, Bbrr], writes=[Blgs])
                yield
                R_ = [Blgs]
                k.op("dve", lambda: V.reduce_max(out=rt[:, 0:1], in_=lgs[:, 0:4], axis=AX.X), reads=R_, writes=[Brt])
                yield
                k.op("dve", lambda: V.tensor_scalar(out=rt[:, 1:2], in0=rt[:, 0:1], scalar1=-1.0, scalar2=None, op0=ALU.mult), reads=[Brt], writes=[Brt])
                yield
                k.op("act", lambda: A.activation(out=rt[:, 4:8], in_=lgs[:, 0:4], func=AF.Exp, bias=rt[:, 1:2], scale=1.0), reads=[Brt, Blgs], writes=[Brt])
                yield
                k.op("dve", lambda: V.reduce_sum(out=rt[:, 2:3], in_=rt[:, 4:8], axis=AX.X), reads=[Brt], writes=[Brt])
                yield
                k.op("dve", lambda: V.reciprocal(out=rt[:, 3:4], in_=rt[:, 2:3]), reads=[Brt], writes=[Brt])
                yield
                k.op("dve", lambda: V.tensor_scalar(out=rt[:, 8:12], in0=lgs[:, 0:4], scalar1=rt[:, 0:1], scalar2=None, op0=ALU.is_equal),
                     reads=[Brt, Blgs], writes=[Brt])
                yield
                k.op("dve", lambda: V.tensor_tensor(out=rt[:, 16:32].rearrange("p (k j) -> p k j", k=4),
                                                    in0=lgs[:, 4:20].rearrange("p (j k) -> p k j", j=4),
                                                    in1=rt[:, 8:12].unsqueeze(1).to_broadcast([128, 4, 4]), op=ALU.mult),
                     reads=[Brt, Blgs], writes=[Brt])
                yield
                k.op("dve", lambda: V.tensor_reduce(out=rt[:, 12:16], in_=rt[:, 16:32].rearrange("p (k j) -> p k j", k=4), axis=AX.X, op=ALU.add),
                     reads=[Brt], writes=[Brt])
                yield
                k.op("dve", lambda: V.reduce_max(out=rt[:, 32:33], in_=rt[:, 12:16], axis=AX.X), reads=[Brt], writes=[Brt])
                yield
                k.op("dve", lambda: V.tensor_scalar(out=rt[:, 36:40], in0=rt[:, 12:16], scalar1=rt[:, 32:33], scalar2=None, op0=ALU.is_equal),
                     reads=[Brt], writes=[Brt])
                yield
                k.op("dve", lambda: V.scalar_tensor_tensor(out=rt[:, 40:44], in0=rt[:, 36:40], scalar=-1e30, in1=rt[:, 12:16],
                                                          op0=ALU.mult, op1=ALU.add), reads=[Brt], writes=[Brt])
                yield
                k.op("dve", lambda: V.reduce_max(out=rt[:, 33:34], in_=rt[:, 40:44], axis=AX.X), reads=[Brt], writes=[Brt])
                yield
                k.op("dve", lambda: V.tensor_scalar(out=rt[:, 44:48], in0=rt[:, 40:44], scalar1=rt[:, 33:34], scalar2=None, op0=ALU.is_equal),
                     reads=[Brt], writes=[Brt])
                yield
                k.op("dve", lambda: V.tensor_tensor(out=rt[:, 34:35], in0=rt[:, 33:34], in1=rt[:, 32:33], op=ALU.subtract), reads=[Brt], writes=[Brt])
                yield
                k.op("act", lambda: A.activation(out=rt[:, 35:36], in_=rt[:, 34:35], func=AF.Exp), reads=[Brt], writes=[Brt])
                yield
                k.op("dve", lambda: V.tensor_scalar_add(out=rt[:, 48:49], in0=rt[:, 35:36], scalar1=1.0), reads=[Brt], writes=[Brt])
                yield
                k.op("dve", lambda: V.reciprocal(out=rt[:, 49:50], in_=rt[:, 48:49]), reads=[Brt], writes=[Brt])
                yield
                k.op("dve", lambda: V.tensor_tensor(out=rt[:, 50:51], in0=rt[:, 49:50], in1=rt[:, 3:4], op=ALU.mult), reads=[Brt], writes=[Brt])
                yield
                k.op("dve", lambda: V.tensor_tensor(out=rt[:, 51:52], in0=rt[:, 50:51], in1=rt[:, 35:36], op=ALU.mult), reads=[Brt], writes=[Brt])
                yield
                k.op("dve", lambda: V.tensor_scalar(out=rt[:, 52:56], in0=rt[:, 36:40], scalar1=rt[:, 50:51], scalar2=None, op0=ALU.mult),
                     reads=[Brt], writes=[Brt])
                yield
                k.op("dve", lambda: V.scalar_tensor_tensor(out=rt[:, 52:56], in0=rt[:, 44:48], scalar=rt[:, 51:52], in1=rt[:, 52:56],
                                                          op0=ALU.mult, op1=ALU.add), reads=[Brt], writes=[Brt])
                yield
                k.op("dve", lambda: V.tensor_tensor(out=rt[:, 64:80].rearrange("p (j k) -> p j k", j=4),
                                                    in0=rt[:, 8:12].unsqueeze(2).to_broadcast([128, 4, 4]),
                                                    in1=rt[:, 52:56].unsqueeze(1).to_broadcast([128, 4, 4]), op=ALU.mult),
                     reads=[Brt], writes=[Brt])
                yield
                k.op("dve", lambda: V.tensor_copy(out=cmbA[:, i, :], in_=rt[:, 64:80]), reads=[Brt], writes=[BcmbA[i]])
                yield

            KE = int(os.environ.get("KE", "9"))
            for Gi in range(NG if KE >= 2 else 0):
                tokG = slice(Gi * 512, (Gi + 1) * 512)
                for c in range(8):
                    pp, Bpp = nextP()
                    Wqq = Wq_[c // 4]
                    cs = slice((c % 4) * 128, (c % 4 + 1) * 128)

                    def mmq():
                        for kc in range(8):
                            ins = T.matmul(pp[:], lhsT=Wqq[:, kc, cs], rhs=hT[:, kc, tokG], start=(kc == 0), stop=(kc == 7))
                        return ins
                    k.op("pe", mmq, reads=[BhT[Gi], Bw[3 + c // 4]], writes=[Bpp])
                    k.op("act" if c % 2 == 0 else "dve",
                         (lambda: A.copy(out=qTg[:, c, :], in_=pp[:])) if c % 2 == 0 else (lambda: V.tensor_copy(out=qTg[:, c, :], in_=pp[:])),
                         reads=[Bpp], writes=[BqTg])
                def att_head(h):
                    rv, Brv = rinvs[h % 2]
                    for mt in range(2):
                        pp, Bpp = nextP()
                        ms = slice(mt * 128, (mt + 1) * 128)

                        def mms():
                            T.matmul(pp[:], lhsT=kTm[:, 2 * h, ms], rhs=qTg[:, 2 * h, :], start=True, stop=False)
                            return T.matmul(pp[:], lhsT=kTm[:, 2 * h + 1, ms], rhs=qTg[:, 2 * h + 1, :], start=False, stop=True)
                        k.op("pe", mms, reads=[BkTm, BqTg], writes=[Bpp])
                        yield
                        pi = (h % 2) * 2 + mt
                        k.op("act", lambda: A.activation(out=pTm[pi], in_=pp[:], func=AF.Exp, scale=1.0 / 16.0), reads=[Bpp], writes=[BpTm[pi]])
                        yield
                    psum_, Bps_ = nextP()
                    pA = [pTm[(h % 2) * 2], pTm[(h % 2) * 2 + 1]]
                    BpA = [BpTm[(h % 2) * 2], BpTm[(h % 2) * 2 + 1]]

                    def mmsum():
                        T.matmul(psum_[:], lhsT=ones_bf, rhs=pA[0], start=True, stop=False)
                        return T.matmul(psum_[:], lhsT=ones_bf, rhs=pA[1], start=False, stop=True)
                    k.op("pe", mmsum, reads=BpA + [Bcbf], writes=[Bps_])
                    yield
                    k.op("act", lambda: A.activation(out=rv, in_=psum_[:], func=AF.Ln), reads=[Bps_], writes=[Brv])
                    yield
                    k.op("act", lambda: A.activation(out=rv, in_=rv, func=AF.Exp, scale=-1.0), reads=[Brv], writes=[Brv])
                    yield
                    for jj in range(2):
                        c = 2 * h + jj
                        pp, Bpp = nextP()

                        def mmpv():
                            T.matmul(pp[:], lhsT=vm[:, 0, c * 128:(c + 1) * 128], rhs=pA[0], start=True, stop=False)
                            return T.matmul(pp[:], lhsT=vm[:, 1, c * 128:(c + 1) * 128], rhs=pA[1], start=False, stop=True)
                        k.op("pe", mmpv, reads=BpA + [Bvm], writes=[Bpp])
                        yield
                        k.op("dve", lambda: V.tensor_tensor(out=oTg[:, c, :], in0=pp[:], in1=rv, op=ALU.mult), reads=[Bpp, Brv], writes=[BoTg])
                        yield
                for hh in (0, 2):
                    gens = [att_head(hh), att_head(hh + 1)]
                    while gens:
                        for g_ in list(gens):
                            try:
                                next(g_)
                            except StopIteration:
                                gens.remove(g_)
                def e_a(ti):
                    i = Gi * 4 + ti
                    ts_ = slice(ti * 128, (ti + 1) * 128)
                    return resid_a(i, arena[:, i * 1024:(i + 1) * 1024], Bxn[i], lambda kc: oTg[:, kc, ts_], [BoTg], Wo_, (5, 0), 8,
                                   utl[i % 2], Butl[i % 2])
                bk = {0: e_a(0)}
                for ti in range(4):
                    i = Gi * 4 + ti
                    if ti + 1 < 4:
                        bk[ti + 1] = e_a(ti + 1)
                    resid_b(i, bk.pop(ti), utl[i % 2], Butl[i % 2])
                    if ti >= 1:
                        transpose_evac(arena[:, (i - 1) * 1024:i * 1024], Bxn[i - 1], 2, i - 1)
                iL = Gi * 4 + 3
                transpose_evac(arena[:, iL * 1024:(iL + 1) * 1024], Bxn[iL], 2, iL)
                gens = [router_tile(Gi * 4 + j, Gi, rtb[j][:, 0:80], Brtb[j], rtb[j][:, 80:100], Blgb[j]) for j in range(4)]
                while gens:
                    for g_ in list(gens):
                        try:
                            next(g_)
                        except StopIteration:
                            gens.remove(g_)
            k.barrier()
            if stop == "E":
                dump_and_stop("arena")
                break

            load_resid_consts(2)
            o = 0
            hid = [carve(4096, BF16).rearrange("p (c t) -> p c t", c=4) for _ in range(2)]
            Bhid = [Buf(), Buf()]
            fr = []
            for r in range(2):
                d = dict(sa=carve(2048, F32), t=carve(2048, F32))
                d["B"] = {nm: Buf() for nm in ("sa", "t")}
                fr.append(d)
            cmbs = [carve(2048, F32) for _ in range(2)]
            Bcmbs = [Buf(), Buf()]
            cbc = [carve(512, F32) for _ in range(2)]
            Bcbc = [Buf(), Buf()]
            outr = [carve(4096, F32), carve(4096, F32)]
            Boutr = [Buf(), Buf()]
            g3bc = carve(4096, F32)
            b3bc = carve(4096, F32)
            assert o <= SCR, o
            k.op("sp", lambda: SPQ.dma_start(out=g3bc, in_=rowv_d[3:4, :].partition_broadcast(128)), writes=[Bg3], dma=dg3)
            k.op("sp", lambda: SPQ.dma_start(out=b3bc, in_=rowv_d[4:5, :].partition_broadcast(128)), writes=[Bb3], dma=db3)
            for i in range(NT):
                xo = arena[:, i * 1024:(i + 1) * 1024]
                k.op("dve", lambda: V.tensor_tensor(out=xo, in0=xo, in1=agbc[:], op=ALU.mult), reads=[Bxn[i], Bagbc], writes=[Bxn[i]])
                k.op("dve", lambda: V.tensor_tensor(out=xo, in0=xo, in1=abbc[:], op=ALU.add), reads=[Bxn[i], Babbc], writes=[Bxn[i]])

            def load_expert(e):
                base = (e % 2) * 3
                return (wload(base, w1_d[e], 8), wload(base + 1, w3_d[e], 8), wload(base + 2, w2_d[e], 4), base)
            PA = [(P[0], BP[0]), (P[1], BP[1])]
            PB = [(P[2], BP[2]), (P[3], BP[3])]
            PY = [(P[4], BP[4]), (P[5], BP[5]), (P[6], BP[6])]
            PC = (P[7], BP[7])
            Wcur = load_expert(0)
            fc = 0
            yc = 0
            gc_ = 0
            pend = None

            def emit_y(pd, last=False):
                nonlocal yc
                H, gi2, Gi, W2, base = pd
                for ti in range(4):
                    i = Gi * 4 + ti
                    ts_ = slice(ti * 128, (ti + 1) * 128)
                    xo = arena[:, i * 1024:(i + 1) * 1024]
                    for n in range(2):
                        ns = slice(n * 512, (n + 1) * 512)
                        py, Bpy = PY[yc % 3]
                        yc += 1

                        def mmy():
                            for kc in range(4):
                                ins = T.matmul(py[:], lhsT=H[:, kc, ts_], rhs=W2[:, kc, ns], start=(kc == 0), stop=(kc == 3))
                            return ins
                        k.op("pe", mmy, reads=[Bhid[gi2], Bw[base + 2]], writes=[Bpy])
                        k.op("dve", lambda: V.tensor_tensor(out=xo[:, ns], in0=xo[:, ns], in1=py[:], op=ALU.add), reads=[Bxn[i], Bpy], writes=[Bxn[i]])
                    if last:
                        ln3_group(Gi, only=i)
            cbn = [0]

            def cmb_prep(e_, G_i, g2):
                for ti in range(4):
                    i = G_i * 4 + ti
                    cbi = cbn[0] % 2
                    cbn[0] += 1
                    k.op("act", lambda: A.copy(out=cbc[cbi], in_=cmbA[:, i, e_:e_ + 1].to_broadcast([128, 128])), reads=[BcmbA[i]], writes=[Bcbc[cbi]])
                    k.op("pe", lambda: T.matmul(PC[0][:, ti * 128:(ti + 1) * 128], lhsT=cbc[cbi], rhs=idf, start=True, stop=True),
                         reads=[Bcbc[cbi], Bcst], writes=[PC[1]])
                k.op("act", lambda: A.copy(out=cmbs[g2], in_=PC[0][:]), reads=[PC[1]], writes=[Bcmbs[g2]])

            def ln3_group(Gq, only=None):
                for i in (range(Gq * 4, Gq * 4 + 4) if only is None else [only]):
                    s_ = i % 2
                    xo = arena[:, i * 1024:(i + 1) * 1024]
                    rstd, nmr, B = ln_stats(xo, Bxn[i])
                    k.op("act", lambda: A.activation(out=outr[s_], in_=xo, func=AF.Identity, scale=rstd, bias=nmr), reads=[Bxn[i], B], writes=[Boutr[s_]])
                    k.op("dve", lambda: V.tensor_tensor(out=outr[s_], in0=outr[s_], in1=g3bc, op=ALU.mult), reads=[Boutr[s_], Bg3], writes=[Boutr[s_]])
                    k.op("dve", lambda: V.tensor_tensor(out=outr[s_], in0=outr[s_], in1=b3bc, op=ALU.add), reads=[Boutr[s_], Bb3], writes=[Boutr[s_]])
                    k.op("sp", lambda: SPQ.dma_start(out=y[b, i * 128:(i + 1) * 128, :], in_=outr[s_]), reads=[Boutr[s_]], dma=douts[s_])
            for e in range(16):
                W1, W3, W2, base = Wcur
                Wnext = None
                for Gi in range(NG):
                    tokG = slice(Gi * 512, (Gi + 1) * 512)
                    gi2 = gc_ % 2
                    if gc_ == 0:
                        cmb_prep(0, 0, 0)
                    gc_ += 1
                    H = hid[gi2]
                    for c in range(4):
                        Fr = fr[fc % 2]
                        FB = Fr["B"]
                        pa, Bpa = PA[fc % 2]
                        pb_, Bpb_ = PB[fc % 2]
                        fc += 1
                        cs = slice(c * 128, (c + 1) * 128)

                        def mmab(pp, W):
                            for kc in range(8):
                                ins = T.matmul(pp[:], lhsT=W[:, kc, cs], rhs=hT[:, kc, tokG], start=(kc == 0), stop=(kc == 7))
                            return ins
                        k.op("pe", lambda: mmab(pa, W1), reads=[BhT[Gi], Bw[base]], writes=[Bpa])
                        k.op("pe", lambda: mmab(pb_, W3), reads=[BhT[Gi], Bw[base + 1]], writes=[Bpb_])
                        if c == 0:
                            ne, nG = (e, Gi + 1) if Gi + 1 < NG else (e + 1, 0)
                            if ne < 16:
                                cmb_prep(ne, nG, gc_ % 2)
                        k.op("act", lambda: A.activation(out=Fr["sa"], in_=pa[:], func=AF.Silu), reads=[Bpa], writes=[FB["sa"]])
                        k.op("dve", lambda: V.tensor_tensor(out=Fr["t"], in0=Fr["sa"], in1=pb_[:], op=ALU.mult), reads=[FB["sa"], Bpb_], writes=[FB["t"]])
                        k.op("dve", lambda: V.tensor_tensor(out=H[:, c, :], in0=Fr["t"], in1=cmbs[gi2], op=ALU.mult),
                             reads=[FB["t"], Bcmbs[gi2]], writes=[Bhid[gi2]])
                    if pend is not None:
                        emit_y(pend[:5], last=(pend[5] == 15))
                    pend = (H, gi2, Gi, W2, base, e)
                    if Gi == 0 and e + 1 < 16:
                        Wnext = load_expert(e + 1)
                Wcur = Wnext
            emit_y(pend[:5], last=True)
            k.barrier()
        for kk, v in k.cnt.items():
            if v > 0:
                SPQ.wait_ge(k.sems[kk], v)
    return nc


def prep_inputs(inp, nb_per_core, ncores):
    f = lambda a: np.ascontiguousarray(np.asarray(a, dtype=np.float32))
    colv = np.zeros((128, 56), np.float32)
    for l, (g, bb) in enumerate(((inp["ln_in_g"], inp["ln_in_b"]), (inp["ln1_g"][0], inp["ln1_b"][0]), (inp["ln2_g"][0], inp["ln2_b"][0]))):
        colv[:, l * 16:l * 16 + 8] = np.asarray(g).reshape(8, 128).T
        colv[:, l * 16 + 8:l * 16 + 16] = np.asarray(bb).reshape(8, 128).T
    lbl = np.asarray(inp["hg_lb_logits"])
    colv[:, 48:52] = lbl[0].reshape(4, 128).T
    colv[:, 52:56] = lbl[1].reshape(4, 128).T
    rowv = np.stack([np.asarray(inp["ln_in_g"]), np.asarray(inp["ln1_g"][0]), np.asarray(inp["ln2_g"][0]), np.asarray(inp["ln3_g"][0]),
                     np.asarray(inp["ln3_b"][0]), np.asarray(inp["ln_in_b"]), np.asarray(inp["ln1_b"][0]), np.asarray(inp["ln2_b"][0])]).astype(np.float32)
    shared = {
        "w_in": f(inp["w_in"][0]), "w_branch_a": f(inp["w_branch_a"][0]), "w_branch_b": f(inp["w_branch_b"][0]),
        "w_mix_out": f(inp["w_mix_out"][0]), "xa_wq": f(inp["xa_wq"][0]), "xa_wk": f(inp["xa_wk"][0]), "xa_wv": f(inp["xa_wv"][0]),
        "xa_wo": f(inp["xa_wo"][0]), "moe_w1": f(inp["moe_w1"][0]), "moe_w3": f(inp["moe_w3"][0]), "moe_w2": f(inp["moe_w2"][0]),
        "cst": make_consts(), "colv": colv, "rowv": f(rowv), "ng": f(np.asarray(inp["hg_norm_g"]).reshape(1, 128)),
        "wr": f(np.concatenate([np.asarray(inp["router_wg"][0]), np.asarray(inp["router_we"][0])], axis=1)),
        "br": f(np.concatenate([np.asarray(inp["router_bg"][0]), np.asarray(inp["router_be"][0])]).reshape(1, 20)),
    }
    xs = f(inp["x"])
    ms = f(inp["mem"])
    maps = []
    for c in range(ncores):
        m = dict(shared)
        m["x"] = xs[c * nb_per_core:(c + 1) * nb_per_core]
        m["mem"] = ms[c * nb_per_core:(c + 1) * nb_per_core]
        maps.append(m)
    return maps


def kernel(**inputs):
    B, S, _ = inputs["x"].shape
    ncores = 8
    nb = B // ncores
    nc = build(S=S, NB=nb)
    maps = prep_inputs(inputs, nb, ncores)
    res = run_bass_kernel_spmd(nc, maps, core_ids=list(range(ncores)))
    return np.concatenate([r["y"] for r in res.results], axis=0).astype(np.float32)
```
